# Optimizing a Trainium2 kernel written in Bass

```python
import math
import jax, jax.numpy as jnp
from jax import lax
import numpy as np

D_MODEL = 2048
BATCH = 4
SEQ = 4096
DEPTH = 2

DN_HEAD_DIM = 128
DN_HEADS = (D_MODEL // 2) // DN_HEAD_DIM
DN_WIDTH = DN_HEADS * DN_HEAD_DIM
CONV_WIDTH = 4
CHUNK = 64
POOL_WINDOWS = (2, 4, 8, 16)
POOL_GROUPS = len(POOL_WINDOWS)
POOL_WIDTH = D_MODEL - DN_WIDTH
POOL_GROUP_DIM = POOL_WIDTH // POOL_GROUPS
MIX_WIDTH = DN_WIDTH + POOL_WIDTH
IN_COLS = 4 * DN_WIDTH + 2 * DN_HEADS + POOL_WIDTH
N_EXPERTS = 16
N_GROUPS = 4
EXPERTS_PER_GROUP = N_EXPERTS // N_GROUPS
TOP_K = 2
D_EXPERT = 1024
DISPATCH_BLOCK = 128
PLE_DIM = 256
ALPHA = (2.0 * DEPTH) ** 0.25
BETA_INIT = (8.0 * DEPTH) ** -0.25
LN_EPS = 1e-5
RMS_EPS = 1e-6

kernel_name = "hybrid_deltanet_pool_groupmoe_deepnorm"


def layer_norm(x, g, b):
    xf = x.astype(jnp.float32)
    mu = jnp.mean(xf, -1, keepdims=True)
    var = jnp.mean(jnp.square(xf - mu), -1, keepdims=True)
    y = (xf - mu) * lax.rsqrt(var + LN_EPS) * g.astype(jnp.float32) + b.astype(jnp.float32)
    return y.astype(x.dtype)


def l2norm(t):
    return t * lax.rsqrt(jnp.sum(t * t, -1, keepdims=True) + RMS_EPS)


def causal_conv_silu(x, w):
    S = x.shape[1]
    xp = jnp.pad(x, ((0, 0), (CONV_WIDTH - 1, 0), (0, 0)))
    y = xp[:, 0:S] * w[0]
    for j in range(1, CONV_WIDTH):
        y = y + xp[:, j:j + S] * w[j]
    return jax.nn.silu(y)


def chunk_gated_delta_rule(q, k, v, beta, g):
    B, S, H, Dk = q.shape
    Dv = v.shape[-1]
    N = S // CHUNK

    def to_chunks(t):
        t = jnp.moveaxis(t, 2, 1)
        return t.reshape((B, H, N, CHUNK) + t.shape[3:])

    q = to_chunks(l2norm(q) * (Dk ** -0.5))
    k = to_chunks(l2norm(k))
    v = to_chunks(v)
    beta = to_chunks(beta)
    g = to_chunks(g)
    gam = jnp.cumsum(g, -1)
    ci = jnp.arange(CHUNK)
    incl = ci[:, None] >= ci[None, :]
    strict = ci[:, None] > ci[None, :]
    decay = jnp.exp(jnp.where(incl, gam[..., :, None] - gam[..., None, :], -jnp.inf))
    kb = k * beta[..., None]
    a = jnp.where(strict, jnp.einsum('bhnid,bhnjd->bhnij', kb, k) * decay, 0.0)
    lhs = a + jnp.eye(CHUNK, dtype=a.dtype)
    rhs = jnp.concatenate([v * beta[..., None], kb * jnp.exp(gam)[..., None]], -1)
    sol = lax.linalg.triangular_solve(lhs, rhs, left_side=True, lower=True, unit_diagonal=True)
    u, w = sol[..., :Dv], sol[..., Dv:]
    attn_qk = jnp.einsum('bhnid,bhnjd->bhnij', q, k) * decay
    q_dec = q * jnp.exp(gam)[..., None]
    k_dec = k * jnp.exp(gam[..., -1:] - gam)[..., None]
    chunk_decay = jnp.exp(gam[..., -1])

    def step(state, inp):
        qd, kd, wc, uc, aqk, cd = inp
        v_new = uc - jnp.einsum('bhck,bhkv->bhcv', wc, state)
        o = jnp.einsum('bhck,bhkv->bhcv', qd, state) + jnp.einsum('bhij,bhjv->bhiv', aqk, v_new)
        state = state * cd[..., None, None] + jnp.einsum('bhck,bhcv->bhkv', kd, v_new)
        return state, o

    xs = (jnp.moveaxis(q_dec, 2, 0), jnp.moveaxis(k_dec, 2, 0), jnp.moveaxis(w, 2, 0),
          jnp.moveaxis(u, 2, 0), jnp.moveaxis(attn_qk, 2, 0), jnp.moveaxis(chunk_decay, 2, 0))
    state0 = jnp.zeros((B, H, Dk, Dv), jnp.float32)
    _, o = lax.scan(step, state0, xs)
    o = jnp.moveaxis(o, 0, 2).reshape(B, H, S, Dv)
    return jnp.moveaxis(o, 1, 2)


def gated_deltanet_group(qkv_raw, z, b_raw, a_raw, conv_w, a_log, dt_bias, norm_w):
    B, S, _ = qkv_raw.shape
    qkv = causal_conv_silu(qkv_raw, conv_w).astype(jnp.float32)
    q, k, v = jnp.split(qkv, 3, axis=-1)
    q = q.reshape(B, S, DN_HEADS, DN_HEAD_DIM)
    k = k.reshape(B, S, DN_HEADS, DN_HEAD_DIM)
    v = v.reshape(B, S, DN_HEADS, DN_HEAD_DIM)
    beta = jax.nn.sigmoid(b_raw.astype(jnp.float32))
    g = -jnp.exp(a_log.astype(jnp.float32)) * jax.nn.softplus(a_raw.astype(jnp.float32) + dt_bias.astype(jnp.float32))
    o = chunk_gated_delta_rule(q, k, v, beta, g)
    o = o * lax.rsqrt(jnp.mean(o * o, -1, keepdims=True) + RMS_EPS) * norm_w.astype(jnp.float32)
    o = o * jax.nn.silu(z.astype(jnp.float32).reshape(B, S, DN_HEADS, DN_HEAD_DIM))
    return o.reshape(B, S, DN_WIDTH).astype(qkv_raw.dtype)


def multiscale_pool_group(u, w_pool, scale):
    B, S, _ = u.shape
    ug = u.astype(jnp.float32).reshape(B, S, POOL_GROUPS, POOL_GROUP_DIM)
    cs = jnp.cumsum(ug, axis=1)
    t = jnp.arange(S)
    outs = []
    for gi, win in enumerate(POOL_WINDOWS):
        csg = cs[:, :, gi]
        shifted = jnp.pad(csg[:, :S - win], ((0, 0), (win, 0), (0, 0)))
        cnt = jnp.minimum(t + 1, win).astype(jnp.float32)
        outs.append((csg - shifted) / cnt[None, :, None] - ug[:, :, gi])
    d = jnp.stack(outs, axis=2)
    y = jnp.einsum('bsgc,gcd->bsgd', d, w_pool.astype(jnp.float32))
    return (y.reshape(B, S, POOL_WIDTH) * scale.astype(jnp.float32)).astype(u.dtype)


def grouped_top2_moe(h, w_router, b_router, w_gate, w_up, w_down):
    T, D = h.shape
    TK = T * TOP_K
    logits = h.astype(jnp.float32) @ w_router.astype(jnp.float32) + b_router.astype(jnp.float32)
    probs = jax.nn.softmax(logits, -1).reshape(T, N_GROUPS, EXPERTS_PER_GROUP)
    group_score = jnp.sum(lax.top_k(probs, TOP_K)[0], -1)
    g_sel = jnp.argmax(group_score, -1)
    in_group = jnp.take_along_axis(probs, g_sel[:, None, None], axis=1)[:, 0]
    top_p, top_i = lax.top_k(in_group, TOP_K)
    expert_idx = g_sel[:, None] * EXPERTS_PER_GROUP + top_i
    gate = top_p / jnp.sum(top_p, -1, keepdims=True)

    flat_e = expert_idx.reshape(-1).astype(jnp.int32)
    flat_tok = jnp.repeat(jnp.arange(T, dtype=jnp.int32), TOP_K)
    flat_w = gate.reshape(-1)
    order = jnp.argsort(flat_e)
    se = flat_e[order]
    counts = jnp.bincount(flat_e, length=N_EXPERTS).astype(jnp.int32)
    start = jnp.cumsum(counts) - counts
    pcounts = (counts + DISPATCH_BLOCK - 1) // DISPATCH_BLOCK * DISPATCH_BLOCK
    pend = jnp.cumsum(pcounts)
    pstart = pend - pcounts
    dest = pstart[se] + jnp.arange(TK, dtype=jnp.int32) - start[se]
    n_rows = TK + N_EXPERTS * DISPATCH_BLOCK
    n_blocks = n_rows // DISPATCH_BLOCK
    src_tok = jnp.full((n_rows,), T, jnp.int32).at[dest].set(flat_tok[order])
    src_w = jnp.zeros((n_rows,), jnp.float32).at[dest].set(flat_w[order])
    block_start = jnp.arange(n_blocks, dtype=jnp.int32) * DISPATCH_BLOCK
    block_e = jnp.minimum(jnp.searchsorted(pend, block_start, side='right'), N_EXPERTS - 1)

    h_pad = jnp.concatenate([h, jnp.zeros((1, D), h.dtype)], 0)
    x_rows = h_pad[src_tok].reshape(n_blocks, DISPATCH_BLOCK, D)

    def expert_rows(args):
        xb, e = args
        hid = jax.nn.silu(xb @ w_gate[e]) * (xb @ w_up[e])
        return hid @ w_down[e]

    y_rows = lax.map(expert_rows, (x_rows, block_e)).reshape(n_rows, D)
    y_rows = y_rows * src_w[:, None].astype(y_rows.dtype)
    out = jax.ops.segment_sum(y_rows, src_tok, num_segments=T + 1)
    return out[:T]


def setup_inputs(seed: int = 0) -> dict:
    key = jax.random.key(seed)
    ks = jax.random.split(key, 24)
    f32 = jnp.float32
    nrm = lambda k, shape: jax.random.normal(k, shape, f32)
    dt = jnp.exp(jax.random.uniform(ks[5], (DEPTH, DN_HEADS), f32, math.log(1e-3), math.log(1e-1)))
    return {
        "x": nrm(ks[0], (BATCH, SEQ, D_MODEL)),
        "p": nrm(ks[1], (DEPTH, BATCH, SEQ, PLE_DIM)),
        "w_in": nrm(ks[2], (DEPTH, D_MODEL, IN_COLS)) * D_MODEL ** -0.5,
        "conv_w": nrm(ks[3], (DEPTH, CONV_WIDTH, 3 * DN_WIDTH)) * CONV_WIDTH ** -0.5,
        "a_log": jnp.log(jax.random.uniform(ks[4], (DEPTH, DN_HEADS), f32, 1.0, 16.0)),
        "dt_bias": dt + jnp.log(-jnp.expm1(-dt)),
        "dn_norm_w": 1.0 + 0.02 * nrm(ks[6], (DEPTH, DN_HEAD_DIM)),
        "pool_w": nrm(ks[7], (DEPTH, POOL_GROUPS, POOL_GROUP_DIM, POOL_GROUP_DIM)) * POOL_GROUP_DIM ** -0.5,
        "pool_scale": 1.0 + 0.1 * nrm(ks[8], (DEPTH, POOL_WIDTH)),
        "w_out": nrm(ks[9], (DEPTH, MIX_WIDTH, D_MODEL)) * (MIX_WIDTH ** -0.5 * BETA_INIT),
        "ln1_g": 1.0 + 0.02 * nrm(ks[10], (DEPTH, D_MODEL)),
        "ln1_b": 0.02 * nrm(ks[11], (DEPTH, D_MODEL)),
        "w_router": nrm(ks[12], (D_MODEL, N_EXPERTS)) * D_MODEL ** -0.5,
        "b_router": 0.01 * nrm(ks[13], (N_EXPERTS,)),
        "w_e_gate": nrm(ks[14], (DEPTH, N_EXPERTS, D_MODEL, D_EXPERT)) * D_MODEL ** -0.5,
        "w_e_up": nrm(ks[15], (DEPTH, N_EXPERTS, D_MODEL, D_EXPERT)) * D_MODEL ** -0.5,
        "w_e_down": nrm(ks[16], (DEPTH, N_EXPERTS, D_EXPERT, D_MODEL)) * (D_EXPERT ** -0.5 * BETA_INIT),
        "ln2_g": 1.0 + 0.02 * nrm(ks[17], (DEPTH, D_MODEL)),
        "ln2_b": 0.02 * nrm(ks[18], (DEPTH, D_MODEL)),
        "w_ple_proj": nrm(ks[19], (DEPTH, PLE_DIM, D_MODEL)) * (PLE_DIM ** -0.5 * BETA_INIT),
        "w_ple_gate": nrm(ks[20], (DEPTH, D_MODEL, D_MODEL)) * D_MODEL ** -0.5,
    }


def reference(x, p, w_in, conv_w, a_log, dt_bias, dn_norm_w, pool_w, pool_scale, w_out,
              ln1_g, ln1_b, w_router, b_router, w_e_gate, w_e_up, w_e_down, ln2_g, ln2_b,
              w_ple_proj, w_ple_gate):
    B, S, D = x.shape
    splits = [3 * DN_WIDTH, 4 * DN_WIDTH, 4 * DN_WIDTH + DN_HEADS, 4 * DN_WIDTH + 2 * DN_HEADS]
    for i in range(DEPTH):
        proj = x @ w_in[i]
        qkv_raw, z, b_raw, a_raw, u = jnp.split(proj, splits, axis=-1)
        y_dn = gated_deltanet_group(qkv_raw, z, b_raw, a_raw, conv_w[i], a_log[i], dt_bias[i], dn_norm_w[i])
        y_pool = multiscale_pool_group(u, pool_w[i], pool_scale[i])
        mix = jnp.concatenate([y_dn, y_pool], -1) @ w_out[i]
        x = layer_norm(ALPHA * x + mix, ln1_g[i], ln1_b[i])
        ffn = grouped_top2_moe(x.reshape(B * S, D), w_router, b_router,
                               w_e_gate[i], w_e_up[i], w_e_down[i]).reshape(B, S, D)
        x = layer_norm(ALPHA * x + ffn, ln2_g[i], ln2_b[i])
        x = x + jax.nn.sigmoid(x @ w_ple_gate[i]) * (p[i] @ w_ple_proj[i])
    return x
```

```python
import contextlib
import os
LV = int(os.environ.get("LV", "9"))
import numpy as np
import concourse.bass as bass
import concourse.mybir as mybir
from concourse.bass_utils import run_bass_kernel_spmd

F32 = mybir.dt.float32
F32R = mybir.dt.float32r
AF = mybir.ActivationFunctionType
ALU = mybir.AluOpType
AX = mybir.AxisListType

D = 2048
DEPTH = 2
BATCH = 4
SEQ = 4096
NEXP = 16
DEXP = 1024
ALPHA = (2.0 * DEPTH) ** 0.25
LN_EPS = 1e-5
RMS_EPS = 1e-6
POOL_WINDOWS = (2, 4, 8, 16)


class Prog:
    ENGS = ("pe", "act", "dve", "pool", "sp")

    def __init__(self, nc):
        self.nc = nc
        self.stack = contextlib.ExitStack()
        self.ops = {e: [] for e in self.ENGS}
        self.cnt = {e: 0 for e in self.ENGS}
        self.esem = {}
        for e in self.ENGS:
            self.esem[e] = self.stack.enter_context(nc.semaphore("sem_" + e))
        self.dsem = {}
        self.res = {}
        self.seen = {e: {} for e in self.ENGS}

    def sb(self, name, shape, dtype=F32):
        return self.stack.enter_context(self.nc.sbuf_tensor("s_" + name, list(shape), dtype))

    def ps(self, name, shape, dtype=F32):
        return self.stack.enter_context(self.nc.psum_tensor("p_" + name, list(shape), dtype))

    def _need(self, eng, tok, needs):
        if tok is None:
            return
        sem, val = tok
        if sem is self.esem.get(eng) and eng == "pe":
            return
        k = id(sem)
        if self.seen[eng].get(k, 0) >= val:
            return
        if k not in needs or needs[k][1] < val:
            needs[k] = (sem, val)

    enabled = True

    def op(self, eng, fn, reads=(), writes=(), dma=None):
        if not self.enabled:
            return None
        pk = set(k.split(".")[0] for k in list(reads) + list(writes) if k.startswith("pb"))
        reads = [k for k in reads if not k.startswith("pb")]
        writes = [k for k in writes if not k.startswith("pb")] + sorted(pk)
        needs = {}
        for r in reads:
            ent = self.res.get(r)
            if ent is not None:
                self._need(eng, ent[0], needs)
        for w in writes:
            ent = self.res.get(w)
            if ent is not None:
                self._need(eng, ent[0], needs)
                for tok in ent[1].values():
                    self._need(eng, tok, needs)
        if dma is not None:
            if dma not in self.dsem:
                self.dsem[dma] = [self.stack.enter_context(self.nc.semaphore("dq_" + dma)), 0]
            ds = self.dsem[dma]
            if ds[1] > 0:
                self._need(eng, (ds[0], ds[1]), needs)
            ds[1] += 16
            tok = (ds[0], ds[1])
            inc = (ds[0], 16)
        else:
            self.cnt[eng] += 1
            tok = (self.esem[eng], self.cnt[eng])
            inc = (self.esem[eng], 1)
        waits = list(needs.values())
        for sem, val in waits:
            self.seen[eng][id(sem)] = val
        self.ops[eng].append((waits, fn, inc))
        for r in reads:
            ent = self.res.setdefault(r, [None, {}])
            k = id(tok[0])
            if k not in ent[1] or ent[1][k][1] < tok[1]:
                ent[1][k] = tok
        for w in writes:
            self.res[w] = [tok, {}]
        return tok

    def wait_all(self, eng, keys):
        needs = {}
        for k in keys:
            ent = self.res.get(k)
            if ent is not None:
                self._need(eng, ent[0], needs)
        self.ops[eng].append((list(needs.values()), None, None))

    def emit(self):
        nc = self.nc
        engmap = {"pe": "tensor", "act": "scalar", "dve": "vector", "pool": "gpsimd", "sp": "sync"}
        with nc.Block() as block:
            for e in self.ENGS:
                lst = self.ops[e]

                def body(eng, lst=lst):
                    for waits, fn, inc in lst:
                        for sem, val in waits:
                            eng.wait_ge(sem, val)
                        if fn is not None:
                            fn(eng).then_inc(inc[0], inc[1])

                getattr(block, engmap[e])(body)
        self.stack.close()


class WSlots:
    def __init__(self, P, n, name="W", size=4096):
        self.P = P
        self.n = n
        self.name = name
        self.t = [P.sb("%s%d" % (name, i), [128, size], F32R) for i in range(n)]
        self.i = 0

    def load(self, parts):
        s = self.i % self.n
        self.i += 1
        key = "%s%d" % (self.name, s)
        t = self.t[s]
        for dv, src in parts:
            self.P.op("pool", I("dma_start", out=dv(t), in_=src),
                      writes=[key], dma=key)
        return t, key


def I(m, *a, **k):
    return lambda e: getattr(e, m)(*a, **k)


def pq(b, q):
    return "pb%d.%d" % (b, q)


def pbank(b):
    return [pq(b, q) for q in range(4)]


def build_stage_a(NT):
    nc = bass.Bass("TRN2", target_bir_lowering=False)
    NST = NT // 512
    dr = lambda n, s, k="ExternalInput": nc.dram_tensor(n, list(s), F32, kind=k).ap()
    x = dr("x", [NT, D])
    wA = dr("wA", [D, 2048])
    wU = dr("wU", [D, 512])
    wba = dr("wba", [D, 8])
    convw = dr("convw", [128, 12, 4])
    negA_d = dr("negA", [128, 4])
    dtb_d = dr("dtb", [128, 4])
    normw_d = dr("normw", [128, 1])
    poolw_d = dr("poolw", [2, 256, 256])
    pscale_d = dr("pscale", [128, 4])
    bcur_d = dr("bcur", [2, 128, 128])
    bprev_d = dr("bprev", [2, 128, 128])
    bcur0_d = dr("bcur0", [2, 128, 128])
    ident_d = dr("ident", [128, 128])
    U_d = dr("Umat", [128, 128])
    NEG_d = dr("NEGmat", [128, 128])
    STR_d = dr("STRmat", [128, 128])
    BD_d = dr("BDmat", [128, 128])
    cm_d = dr("cmask", [128, 2])
    mixT = dr("mixT", [1024, NT], "ExternalOutput")
    DBG = int(os.environ.get("DBG", "0"))
    if DBG:
        dbg = dr("dbg", [20, 128, 128], "ExternalOutput")

    P = Prog(nc)
    op = P.op
    consts = {}

    def cload(name, src, shape, dtype=F32, q="sp"):
        t = P.sb(name, shape, dtype)
        if len(shape) == 2:
            op(q, I("dma_start", out=t[:], in_=src), writes=[name], dma="c_" + name)
        else:
            op(q, I("dma_start", out=t[:], in_=src), writes=[name], dma="c_" + name)
        consts[name] = t
        return t

    ident = cload("ident", ident_d, [128, 128])
    Um = cload("Um", U_d, [128, 128])
    NEGm = cload("NEGm", NEG_d, [128, 128])
    STRm = cload("STRm", STR_d, [128, 128])
    BDm = cload("BDm", BD_d, [128, 128])
    cmk = cload("cmk", cm_d, [128, 2])
    cw = cload("cw", convw, [128, 12, 4])
    alog = cload("alog", negA_d, [128, 4])
    dtb = cload("dtb", dtb_d, [128, 4])
    normw = cload("normw", normw_d, [128, 1])
    pscale = cload("pscale", pscale_d, [128, 4])
    bcur = cload("bcur", bcur_d.rearrange("g p n -> p g n"), [128, 2, 128])
    bprev = cload("bprev", bprev_d.rearrange("g p n -> p g n"), [128, 2, 128])
    bcur0 = cload("bcur0", bcur0_d.rearrange("g p n -> p g n"), [128, 2, 128])
    wbat = cload("wbat", wba.rearrange("(c p) n -> p c n", p=128), [128, 16, 8], F32R, q="pool")
    poolw = cload("poolw", poolw_d.rearrange("g (c p) n -> p g c n", p=128), [128, 2, 2, 256], F32R, q="pool")
    ones = P.sb("ones", [128, 128])
    op("dve", I("memset", ones[:], 1.0), writes=["ones"])
    epsc = P.sb("epsc", [128, 2])
    op("dve", I("memset", epsc[:, 0:1], RMS_EPS), writes=["epsc"])
    negA = P.sb("negA", [128, 4])
    op("act", I("activation", out=negA[:], in_=alog[:], func=AF.Exp), reads=["alog"], writes=["negA"])
    op("dve", I("tensor_scalar", out=negA[:], in0=negA[:], scalar1=-1.0, scalar2=None, op0=ALU.mult),
       reads=["negA"], writes=["negA"])

    xtok = [P.sb("xtok%d" % i, [128, D]) for i in range(2)]
    xT = P.sb("xT", [128, 16, 512], F32R)
    W = WSlots(P, 3, size=2048)
    raw = P.sb("raw", [128, 12, 515])
    cq = P.sb("cq", [128, 12, 512])
    szT = P.sb("szT", [128, 4, 512])
    ut = P.sb("utok", [128, 4, 512])
    utk = "utok"
    uprev = P.sb("uprev", [128, 512])
    dT = P.sb("dT", [128, 4, 512], F32R)
    poolst = P.sb("poolst", [128, 4, 512])
    oT = P.sb("oT", [128, 4, 512])
    tmpa = P.sb("tmpa", [128, 512])
    tmpb = P.sb("tmpb", [128, 512])
    S = [P.sb("S%d" % h, [128, 128]) for h in range(4)]
    sms = [P.sb("sm%d" % i, [128, 64]) for i in range(2)]
    PREP = ["gU", "E", "Es", "kg", "vtok", "X0", "X1", "Y0", "Y1", "R0", "R1", "tD"]
    REC = ["qdec", "aqk", "kdec", "u0b", "w0t", "vnew"]
    hbt = {}
    for nm in PREP:
        for hp in range(2):
            hbt[(nm, hp)] = P.sb("%s_%d" % (nm, hp), [128, 128])
    for nm in REC:
        for h in range(4):
            hbt[(nm, h)] = P.sb("%s_%d" % (nm, h), [128, 128])

    def HT(nm, h):
        return hbt[(nm, h % 2)] if nm in PREP else hbt[(nm, h)]

    def HK(nm, h):
        return "%s_%d" % (nm, h % 2) if nm in PREP else "%s_%d" % (nm, h)

    pb = [P.ps("pb%d" % i, [128, 512]) for i in range(8)]
    HB = [0, 1, 4, 7]

    for h in range(4):
        op("dve", I("memset", S[h][:], 0.0), writes=["S%d" % h])
    op("dve", I("memset", raw[:], 0.0), writes=["raw%d" % bi for bi in range(12)])

    for st in range(NST):
        t0 = st * 512
        P.enabled = True
        for tt in range(4):
            xk = "xtok%d" % (tt % 2)
            xtt = xtok[tt % 2]
            op("sp", I("dma_start", out=xtt[:], in_=x[t0 + tt * 128:t0 + (tt + 1) * 128, :]), writes=[xk], dma=xk)
            for cg in range(4):
                for c4 in range(4):
                    c = cg * 4 + c4
                    op("pe", I("transpose", pb[3][:, c4 * 128:(c4 + 1) * 128], xtt[:, c * 128:(c + 1) * 128], ident[:]),
                       reads=[xk, "ident"], writes=[pq(3, c4)])
                src = pb[3][:, :].rearrange("p (c n) -> p c n", c=4)
                dst = xT[:, cg * 4:(cg + 1) * 4, tt * 128:(tt + 1) * 128]
                if cg % 2 == 0:
                    op("act", I("activation", out=dst, in_=src, func=AF.Copy), reads=pbank(3), writes=["xT"])
                else:
                    op("dve", I("tensor_copy", out=dst, in_=src), reads=pbank(3), writes=["xT"])
        for blk in range(16):
            wt, wk = W.load([(lambda t: t[:, :].rearrange("p (c n) -> p c n", c=16),
                              wA[:, blk * 128:(blk + 1) * 128].rearrange("(c p) n -> p c n", p=128))])
            wv = wt[:, :].rearrange("p (c n) -> p c n", c=16)
            h, kind = blk // 4, blk % 4
            bank = blk % 2
            for c in range(16):
                op("pe", I("matmul", pb[bank][:, :], lhsT=wv[:, c, :], rhs=xT[:, c, :], start=(c == 0), stop=(c == 15)),
                   reads=[wk, "xT"], writes=pbank(bank))
            if kind < 3:
                bi = h * 3 + kind
                op("act", I("activation", out=raw[:, bi, 3:515], in_=pb[bank][:, :], func=AF.Copy),
                   reads=pbank(bank), writes=["raw%d" % bi])
            else:
                op("act", I("activation", out=szT[:, h, :], in_=pb[bank][:, :], func=AF.Silu),
                   reads=pbank(bank), writes=["szT"])
        if st > 0:
            op("dve", I("tensor_copy", out=uprev[:], in_=ut[:, 3, :]), reads=[utk], writes=["uprev"])
        for blk in range(4):
            wt, wk = W.load([(lambda t: t[:, :].rearrange("p (c n) -> p c n", c=16),
                              wU[:, blk * 128:(blk + 1) * 128].rearrange("(c p) n -> p c n", p=128))])
            wv = wt[:, :].rearrange("p (c n) -> p c n", c=16)
            for tt in range(4):
                for c in range(16):
                    op("pe", I("matmul", pb[2][:, tt * 128:(tt + 1) * 128], lhsT=xT[:, c, tt * 128:(tt + 1) * 128], rhs=wv[:, c, :], start=(c == 0), stop=(c == 15)),
                       reads=[wk, "xT"], writes=[pq(2, tt)])
            op("act", I("activation", out=ut[:, :, blk * 128:(blk + 1) * 128], in_=pb[2][:, :].rearrange("p (t n) -> p t n", t=4), func=AF.Copy),
               reads=pbank(2), writes=[utk])
        P.enabled = LV >= 2
        for bi in range(12):
            eng = "dve"
            rk = "raw%d" % bi
            op(eng, I("tensor_scalar", out=cq[:, bi, :], in0=raw[:, bi, 0:512], scalar1=cw[:, bi, 0:1], scalar2=None, op0=ALU.mult),
               reads=[rk, "cw"], writes=["cq%d" % bi])
            for j in range(1, 4):
                op(eng, I("scalar_tensor_tensor", out=cq[:, bi, :], in0=raw[:, bi, j:j + 512], scalar=cw[:, bi, j:j + 1], in1=cq[:, bi, :], op0=ALU.mult, op1=ALU.add),
                   reads=[rk, "cw"], writes=["cq%d" % bi])
            op("act", I("activation", out=cq[:, bi, :], in_=cq[:, bi, :], func=AF.Silu), reads=["cq%d" % bi], writes=["cq%d" % bi])
            op(eng, I("tensor_copy", out=raw[:, bi, 0:3], in_=raw[:, bi, 512:515]), reads=[rk], writes=[rk])
        for h in range(4):
            for kind in range(2):
                bi = h * 3 + kind
                ck = "cq%d" % bi
                op("act", I("activation", out=tmpa[:], in_=cq[:, bi, :], func=AF.Square), reads=[ck], writes=["tmpa"])
                op("pe", I("matmul", pb[2][:, :], lhsT=ones[:], rhs=tmpa[:], start=True, stop=True), reads=["ones", "tmpa"], writes=pbank(2))
                op("act", I("activation", out=tmpb[:], in_=pb[2][:, :], func=AF.Sqrt, bias=epsc[:, 0:1]), reads=pbank(2) + ["epsc"], writes=["tmpb"])
                op("dve", I("reciprocal", out=tmpb[:], in_=tmpb[:]), reads=["tmpb"], writes=["tmpb"])
                sc = (128.0 ** -0.5) if kind == 0 else 1.0
                op("dve", I("scalar_tensor_tensor", out=cq[:, bi, :], in0=cq[:, bi, :], scalar=sc, in1=tmpb[:], op0=ALU.mult, op1=ALU.mult),
                   reads=[ck, "tmpb"], writes=[ck])
        P.enabled = LV >= 3
        for tt in range(4):
            gt = st * 4 + tt
            for blk in range(4):
                g = blk // 2
                reg = pq(6, blk)
                dst = pb[6][:, blk * 128:(blk + 1) * 128]
                if gt == 0:
                    op("pe", I("matmul", dst, lhsT=ut[:, tt, blk * 128:(blk + 1) * 128], rhs=bcur0[:, g, :], start=True, stop=True),
                       reads=[utk, "bcur0"], writes=[reg])
                else:
                    if tt == 0:
                        op("pe", I("matmul", dst, lhsT=uprev[:, blk * 128:(blk + 1) * 128], rhs=bprev[:, g, :], start=True, stop=False),
                           reads=["uprev", "bprev"], writes=[reg])
                    else:
                        op("pe", I("matmul", dst, lhsT=ut[:, tt - 1, blk * 128:(blk + 1) * 128], rhs=bprev[:, g, :], start=True, stop=False),
                           reads=[utk, "bprev"], writes=[reg])
                    op("pe", I("matmul", dst, lhsT=ut[:, tt, blk * 128:(blk + 1) * 128], rhs=bcur[:, g, :], start=False, stop=True),
                       reads=[utk, "bcur"], writes=[reg])
            op("dve", I("tensor_copy", out=dT[:, :, tt * 128:(tt + 1) * 128], in_=pb[6][:, :].rearrange("p (b n) -> p b n", b=4)),
               reads=pbank(6), writes=["dT"])
        for g in range(2):
            for oc in range(2):
                blk = g * 2 + oc
                for cc in range(2):
                    op("pe", I("matmul", pb[5][:, :], lhsT=poolw[:, g, cc, oc * 128:(oc + 1) * 128], rhs=dT[:, g * 2 + cc, :], start=(cc == 0), stop=(cc == 1)),
                       reads=["poolw", "dT"], writes=pbank(5))
                op("dve", I("tensor_scalar", out=poolst[:, blk, :], in0=pb[5][:, :], scalar1=pscale[:, blk:blk + 1], scalar2=None, op0=ALU.mult),
                   reads=pbank(5) + ["pscale"], writes=["poolst"])
        op("sp", I("dma_start", out=mixT[512:1024, t0:t0 + 512].rearrange("(b p) t -> p b t", p=128), in_=poolst[:]),
           reads=["poolst"], writes=["mixTp"], dma="poolst")

        P.enabled = LV >= 4
        for tt in range(4):
            par = tt % 2
            c0 = tt * 128
            sm = sms[par]
            smk = "sm%d" % par
            for c in range(16):
                op("pe", I("matmul", pb[2][:, 0:8], lhsT=xT[:, c, c0:c0 + 128], rhs=wbat[:, c, :], start=(c == 0), stop=(c == 15)),
                   reads=["xT", "wbat"], writes=pbank(2))
            op("act", I("activation", out=sm[:, 0:4], in_=pb[2][:, 0:4], func=AF.Sigmoid), reads=pbank(2), writes=[smk])
            op("dve", I("tensor_scalar", out=sm[:, 4:8], in0=sm[:, 0:4], scalar1=-1.0, scalar2=None, op0=ALU.mult), reads=[smk], writes=[smk])
            op("dve", I("tensor_tensor", out=sm[:, 40:44], in0=pb[2][:, 4:8], in1=dtb[:], op=ALU.add), reads=pbank(2) + ["dtb"], writes=[smk])
            op("act", I("activation", out=sm[:, 40:44], in_=sm[:, 40:44], func=AF.Exp), reads=[smk], writes=[smk])
            op("act", I("activation", out=sm[:, 40:44], in_=sm[:, 40:44], func=AF.Ln, bias=1.0), reads=[smk], writes=[smk])
            op("dve", I("tensor_tensor", out=sm[:, 8:12], in0=sm[:, 40:44], in1=negA[:], op=ALU.mult), reads=[smk, "negA"], writes=[smk])
            for ch in range(2):
                op("dve", I("tensor_scalar", out=sm[:, 24 + ch * 4:28 + ch * 4], in0=sm[:, 8:12], scalar1=cmk[:, ch:ch + 1], scalar2=None, op0=ALU.mult),
                   reads=[smk, "cmk"], writes=[smk])
            op("pe", I("matmul", pb[2][:, 8:12], lhsT=Um[:], rhs=sm[:, 8:12], start=True, stop=True), reads=["Um", smk], writes=pbank(2))
            op("pe", I("matmul", pb[2][:, 12:16], lhsT=BDm[:], rhs=sm[:, 8:12], start=True, stop=True), reads=["BDm", smk], writes=pbank(2))
            op("pe", I("matmul", pb[2][:, 16:24], lhsT=ones[:], rhs=sm[:, 24:32], start=True, stop=True), reads=["ones", smk], writes=pbank(2))
            op("dve", I("tensor_copy", out=sm[:, 12:16], in_=pb[2][:, 8:12]), reads=pbank(2), writes=[smk])
            op("act", I("activation", out=sm[:, 16:20], in_=pb[2][:, 8:12], func=AF.Exp), reads=pbank(2), writes=[smk])
            op("dve", I("tensor_tensor", out=sm[:, 20:24], in0=pb[2][:, 12:16], in1=sm[:, 12:16], op=ALU.subtract), reads=pbank(2) + [smk], writes=[smk])
            op("act", I("activation", out=sm[:, 20:24], in_=sm[:, 20:24], func=AF.Exp), reads=[smk], writes=[smk])
            op("act", I("activation", out=sm[:, 32:40], in_=pb[2][:, 16:24], func=AF.Exp), reads=pbank(2), writes=[smk])

            P.enabled = LV >= 5
            for pair in range(2):
                heads = [pair * 2, pair * 2 + 1]
                for h in heads:
                    B = HB[h]
                    qTv = cq[:, h * 3 + 0, c0:c0 + 128]
                    kTv = cq[:, h * 3 + 1, c0:c0 + 128]
                    vTv = cq[:, h * 3 + 2, c0:c0 + 128]
                    qk_, kk_, vk_ = "cq%d" % (h * 3), "cq%d" % (h * 3 + 1), "cq%d" % (h * 3 + 2)
                    T = lambda nm, h=h: HT(nm, h)
                    K = lambda nm, h=h: HK(nm, h)
                    op("pe", I("transpose", pb[B][:, 0:128], kTv, ident[:]), reads=[kk_, "ident"], writes=[pq(B, 0)])
                    op("pe", I("transpose", pb[B][:, 128:256], vTv, ident[:]), reads=[vk_, "ident"], writes=[pq(B, 1)])
                    op("dve", I("tensor_scalar", out=T("kg")[:], in0=pb[B][:, 0:128], scalar1=sm[:, 16 + h:17 + h], scalar2=None, op0=ALU.mult),
                       reads=[pq(B, 0), smk], writes=[K("kg")])
                    op("dve", I("tensor_scalar", out=T("kdec")[:], in0=pb[B][:, 0:128], scalar1=sm[:, 20 + h:21 + h], scalar2=None, op0=ALU.mult),
                       reads=[pq(B, 0), smk], writes=[K("kdec")])
                    op("act", I("activation", out=T("vtok")[:], in_=pb[B][:, 128:256], func=AF.Copy), reads=[pq(B, 1)], writes=[K("vtok")])
                    op("dve", I("tensor_scalar", out=T("gU")[:], in0=Um[:], scalar1=sm[:, 8 + h:9 + h], scalar2=None, op0=ALU.mult),
                       reads=["Um", smk], writes=[K("gU")])
                    op("pe", I("matmul", pb[B][:, 256:384], lhsT=ones[:], rhs=T("gU")[:], start=True, stop=True), reads=["ones", K("gU")], writes=[pq(B, 2)])
                    op("dve", I("scalar_tensor_tensor", out=T("tD")[:], in0=pb[B][:, 256:384], scalar=sm[:, 12 + h:13 + h], in1=NEGm[:], op0=ALU.subtract, op1=ALU.add),
                       reads=[pq(B, 2), smk, "NEGm"], writes=[K("tD")])
                    op("act", I("activation", out=T("E")[:], in_=T("tD")[:], func=AF.Exp), reads=[K("tD")], writes=[K("E")])
                    op("act", I("activation", out=T("tD")[:], in_=pb[B][:, 256:384], func=AF.Exp), reads=[pq(B, 2), K("E")], writes=[K("tD")])
                    op("pool", I("tensor_tensor", out=T("qdec")[:], in0=qTv, in1=T("tD")[:], op=ALU.mult),
                       reads=[qk_, K("tD")], writes=[K("qdec")])
                    op("pool", I("tensor_tensor", out=T("Es")[:], in0=T("E")[:], in1=STRm[:], op=ALU.mult), reads=[K("E"), "STRm"], writes=[K("Es")])
                    op("pe", I("matmul", pb[B][:, 0:128], lhsT=kTv, rhs=kTv, start=True, stop=True), reads=[kk_], writes=[pq(B, 0)])
                    op("pe", I("matmul", pb[B][:, 128:256], lhsT=kTv, rhs=qTv, start=True, stop=True), reads=[kk_, qk_], writes=[pq(B, 1)])
                    op("dve", I("tensor_tensor", out=T("aqk")[:], in0=pb[B][:, 128:256], in1=T("E")[:], op=ALU.mult),
                       reads=[pq(B, 1), K("E")], writes=[K("aqk")])
                    op("dve", I("scalar_tensor_tensor", out=T("X0")[:], in0=pb[B][:, 0:128], scalar=sm[:, 4 + h:5 + h], in1=T("Es")[:], op0=ALU.mult, op1=ALU.mult),
                       reads=[pq(B, 0), smk, K("Es")], writes=[K("X0")])
                    op("pe", I("transpose", pb[B][:, 384:512], T("X0")[:], ident[:]), reads=[K("X0"), "ident"], writes=[pq(B, 3)])
                    op("act", I("activation", out=T("Y0")[:], in_=pb[B][:, 384:512], func=AF.Copy), reads=[pq(B, 3)], writes=[K("Y0")])
                    op("pool", I("tensor_tensor", out=T("R0")[:], in0=T("X0")[:], in1=ident[:], op=ALU.add), reads=[K("X0"), "ident"], writes=[K("R0")])
                for lvl in range(5):
                    a, b = lvl % 2, (lvl + 1) % 2
                    for h in heads:
                        B = HB[h]
                        Xa, Ya, Xb, Yb = HT("X%d" % a, h), HT("Y%d" % a, h), HT("X%d" % b, h), HT("Y%d" % b, h)
                        kXa, kYa, kXb, kYb = HK("X%d" % a, h), HK("Y%d" % a, h), HK("X%d" % b, h), HK("Y%d" % b, h)
                        if lvl < 4:
                            op("pe", I("matmul", pb[B][:, 0:128], lhsT=Ya[:], rhs=Xa[:], start=True, stop=True), reads=[kXa, kYa], writes=[pq(B, 0)])
                        op("pe", I("matmul", pb[B][:, 128:256], lhsT=Xa[:], rhs=Ya[:], start=True, stop=True), reads=[kXa, kYa], writes=[pq(B, 1)])
                        if lvl < 4:
                            op("act", I("activation", out=Xb[:], in_=pb[B][:, 0:128], func=AF.Copy), reads=[pq(B, 0)], writes=[kXb])
                        op("dve", I("tensor_copy", out=Yb[:], in_=pb[B][:, 128:256]), reads=[pq(B, 1)], writes=[kYb])
                    for h in heads:
                        B = HB[h]
                        Yb = HT("Y%d" % b, h)
                        Ra, Rb = HT("R%d" % a, h), HT("R%d" % b, h)
                        kYb, kRa, kRb = HK("Y%d" % b, h), HK("R%d" % a, h), HK("R%d" % b, h)
                        op("pe", I("matmul", pb[B][:, 256:384], lhsT=Yb[:], rhs=Ra[:], start=True, stop=True), reads=[kYb, kRa], writes=[pq(B, 2)])
                        op("dve", I("tensor_tensor", out=Rb[:], in0=pb[B][:, 256:384], in1=Ra[:], op=ALU.add), reads=[pq(B, 2), kRa], writes=[kRb])
                for h in heads:
                    B = HB[h]
                    R = HT("R1", h)
                    kR = HK("R1", h)
                    op("pe", I("matmul", pb[B][:, 384:512], lhsT=R[:], rhs=HT("vtok", h)[:], start=True, stop=True), reads=[kR, HK("vtok", h)], writes=[pq(B, 3)])
                    op("pe", I("matmul", pb[B][:, 0:128], lhsT=HT("kg", h)[:], rhs=R[:], start=True, stop=True), reads=[kR, HK("kg", h)], writes=[pq(B, 0)])
                    op("dve", I("tensor_scalar", out=HT("u0b", h)[:], in0=pb[B][:, 384:512], scalar1=sm[:, h:h + 1], scalar2=None, op0=ALU.mult),
                       reads=[pq(B, 3), smk], writes=[HK("u0b", h)])
                    op("act", I("activation", out=HT("w0t", h)[:], in_=pb[B][:, 0:128], func=AF.Copy), reads=[pq(B, 0)], writes=[HK("w0t", h)])
            P.enabled = LV >= 6
            for ch in range(2):
                r0 = ch * 64
                for h in range(4):
                    rb, rq = 5 + h // 2, (h % 2) * 2
                    op("pe", I("matmul", pb[rb][r0:r0 + 64, rq * 128:(rq + 1) * 128], lhsT=HT("w0t", h)[:, r0:r0 + 64], rhs=S[h][:], start=True, stop=True),
                       reads=[HK("w0t", h), "S%d" % h], writes=[pq(rb, rq)])
                for h in range(4):
                    rb, rq = 5 + h // 2, (h % 2) * 2
                    op("dve", I("scalar_tensor_tensor", out=HT("vnew", h)[r0:r0 + 64, :], in0=pb[rb][r0:r0 + 64, rq * 128:(rq + 1) * 128], scalar=sm[r0:r0 + 64, 4 + h:5 + h], in1=HT("u0b", h)[r0:r0 + 64, :], op0=ALU.mult, op1=ALU.add),
                       reads=[pq(rb, rq), smk, HK("u0b", h)], writes=[HK("vnew", h)])
                for h in range(4):
                    rb, rq = 5 + h // 2, (h % 2) * 2 + 1
                    ob = pq(3, h)
                    op("pe", I("matmul", pb[3][:, h * 128:h * 128 + 64], lhsT=S[h][:], rhs=HT("qdec", h)[:, r0:r0 + 64], start=True, stop=False),
                       reads=["S%d" % h, HK("qdec", h)], writes=[ob])
                    op("pe", I("matmul", pb[3][:, h * 128:h * 128 + 64], lhsT=HT("vnew", h)[r0:r0 + 64, :], rhs=HT("aqk", h)[r0:r0 + 64, r0:r0 + 64], start=False, stop=True),
                       reads=[HK("vnew", h), HK("aqk", h)], writes=[ob])
                    op("pe", I("matmul", pb[rb][:, rq * 128:(rq + 1) * 128], lhsT=HT("kdec", h)[r0:r0 + 64, :], rhs=HT("vnew", h)[r0:r0 + 64, :], start=True, stop=True),
                       reads=[HK("kdec", h), HK("vnew", h)], writes=[pq(rb, rq)])
                for h in range(4):
                    rb, rq = 5 + h // 2, (h % 2) * 2 + 1
                    op("act", I("activation", out=oT[:, h, c0 + r0:c0 + r0 + 64], in_=pb[3][:, h * 128:h * 128 + 64], func=AF.Copy),
                       reads=[pq(3, h)], writes=["oT"])
                    op("dve", I("scalar_tensor_tensor", out=S[h][:], in0=S[h][:], scalar=sm[:, 32 + ch * 4 + h:33 + ch * 4 + h], in1=pb[rb][:, rq * 128:(rq + 1) * 128], op0=ALU.mult, op1=ALU.add),
                       reads=["S%d" % h, smk, pq(rb, rq)], writes=["S%d" % h])
            if DBG and st == 0 and tt == 0:
                P.enabled = True
                items = [(cq[:, 0, 0:128], "cq0"), (cq[:, 1, 0:128], "cq1"), (cq[:, 2, 0:128], "cq2"), (sm[:, :], smk, 64),
                         (HT("E", 0)[:], HK("E", 0)), (HT("X0", 0)[:], HK("X0", 0)), (HT("R1", 0)[:], HK("R1", 0)),
                         (HT("u0b", 0)[:], HK("u0b", 0)), (HT("w0t", 0)[:], HK("w0t", 0)), (HT("qdec", 0)[:], HK("qdec", 0)),
                         (HT("aqk", 0)[:], HK("aqk", 0)), (HT("kdec", 0)[:], HK("kdec", 0)), (HT("vnew", 0)[:], HK("vnew", 0)),
                         (S[0][:], "S0"), (oT[:, 0, 0:128], "oT"), (HT("kg", 0)[:], HK("kg", 0)), (HT("vtok", 0)[:], HK("vtok", 0)),
                         (HT("Y0", 0)[:], HK("Y0", 0))]
                for di, it in enumerate(items):
                    w = it[2] if len(it) > 2 else 128
                    op("sp", I("dma_start", out=dbg[di, :, 0:w], in_=it[0]), reads=[it[1]], writes=["dbg"], dma="dbg")
        P.enabled = LV >= 7
        for h in range(4):
            op("act", I("activation", out=tmpa[:], in_=oT[:, h, :], func=AF.Square), reads=["oT"], writes=["tmpa"])
            op("pe", I("matmul", pb[2][:, :], lhsT=ones[:], rhs=tmpa[:], start=True, stop=True), reads=["ones", "tmpa"], writes=pbank(2))
            op("act", I("activation", out=tmpb[:], in_=pb[2][:, :], func=AF.Sqrt, bias=epsc[:, 0:1], scale=1.0 / 128.0), reads=pbank(2) + ["epsc"], writes=["tmpb"])
            op("dve", I("reciprocal", out=tmpb[:], in_=tmpb[:]), reads=["tmpb"], writes=["tmpb"])
            op("dve", I("scalar_tensor_tensor", out=tmpb[:], in0=oT[:, h, :], scalar=normw[:, 0:1], in1=tmpb[:], op0=ALU.mult, op1=ALU.mult),
               reads=["oT", "normw", "tmpb"], writes=["tmpb"])
            op("dve", I("tensor_tensor", out=oT[:, h, :], in0=tmpb[:], in1=szT[:, h, :], op=ALU.mult), reads=["tmpb", "szT"], writes=["oT"])
        P.enabled = True
        op("sp", I("dma_start", out=mixT[0:512, t0:t0 + 512].rearrange("(b p) t -> p b t", p=128), in_=oT[:]),
           reads=["oT"], writes=["mixTd"], dma="oT")
    P.wait_all("sp", ["mixTd", "mixTp", "dbg"])
    P.emit()
    return nc


def layer_norm_tile(P, src, srck, tmpC, lnb, eps_t, stat):
    op = P.op
    op("dve", I("tensor_reduce", out=stat[:, 0:1], in_=src, axis=AX.X, op=ALU.add), reads=[srck], writes=["stat"])
    op("dve", I("tensor_scalar", out=stat[:, 1:2], in0=stat[:, 0:1], scalar1=1.0 / D, scalar2=None, op0=ALU.mult), reads=["stat"], writes=["stat"])
    op("dve", I("tensor_scalar", out=tmpC[:], in0=src, scalar1=stat[:, 1:2], scalar2=None, op0=ALU.subtract), reads=[srck, "stat"], writes=["tmpC"])
    op("act", I("activation", out=src, in_=tmpC[:], func=AF.Square), reads=["tmpC"], writes=[srck])
    op("dve", I("tensor_reduce", out=stat[:, 2:3], in_=src, axis=AX.X, op=ALU.add), reads=[srck], writes=["stat"])
    op("act", I("activation", out=stat[:, 3:4], in_=stat[:, 2:3], func=AF.Sqrt, bias=eps_t[:, 0:1], scale=1.0 / D), reads=["stat", "epsc"], writes=["stat"])
    op("dve", I("reciprocal", out=stat[:, 3:4], in_=stat[:, 3:4]), reads=["stat"], writes=["stat"])
    op("dve", I("scalar_tensor_tensor", out=tmpC[:], in0=tmpC[:], scalar=stat[:, 3:4], in1=lnb[:, 0, :], op0=ALU.mult, op1=ALU.mult),
       reads=["tmpC", "stat", "lnb"], writes=["tmpC"])
    op("dve", I("tensor_tensor", out=tmpC[:], in0=tmpC[:], in1=lnb[:, 1, :], op=ALU.add), reads=["tmpC", "lnb"], writes=["tmpC"])


def build_stage_b(NTB):
    nc = bass.Bass("TRN2", target_bir_lowering=False)
    NST = NTB // 512
    dr = lambda n, s, k="ExternalInput": nc.dram_tensor(n, list(s), F32, kind=k).ap()
    mixT = dr("mixT", [D, NTB])
    x = dr("x", [NTB, D])
    p = dr("p", [NTB, 256])
    w_out = dr("w_out", [D, D])
    ln1 = dr("ln1", [2, D])
    ln2 = dr("ln2", [2, D])
    w_router = dr("w_router", [D, 16])
    b_router = dr("b_router", [1, 16])
    w_eg = dr("w_eg", [NEXP, D, DEXP])
    w_eu = dr("w_eu", [NEXP, D, DEXP])
    w_ed = dr("w_ed", [NEXP, DEXP, D])
    w_pp = dr("w_pp", [256, D])
    w_pg = dr("w_pg", [D, D])
    ident_d = dr("ident", [128, 128])
    xo = dr("xo", [NTB, D], "ExternalOutput")
    DBG = int(os.environ.get("DBG", "0"))
    if DBG:
        dbg = dr("dbg", [5, 128, D], "ExternalOutput")

    P = Prog(nc)
    op = P.op
    ident = P.sb("ident", [128, 128])
    op("sp", I("dma_start", out=ident[:], in_=ident_d), writes=["ident"], dma="c_ident")
    wr = P.sb("wr", [128, 16, 16], F32R)
    op("pool", I("dma_start", out=wr[:], in_=w_router.rearrange("(c p) n -> p c n", p=128)), writes=["wr"], dma="c_wr")
    brt = P.sb("brt", [128, 16])
    op("sp", I("dma_start", out=brt[:], in_=b_router.partition_broadcast(128)), writes=["brt"], dma="c_brt")

    wppt = P.sb("wppt", [128, 2, D], F32R)
    op("pool", I("dma_start", out=wppt[:], in_=w_pp.rearrange("(c p) n -> p c n", p=128)), writes=["wppt"], dma="c_wppt")
    bufA = P.sb("bufA", [128, 16, 512], F32R)
    bufB = P.sb("bufB", [128, 4, D])
    xt = P.sb("xt", [128, D])
    tmpC = P.sb("tmpC", [128, D])
    lnb = P.sb("lnb", [128, 2, D])
    hid = P.sb("hid", [128, 8, 512], F32R)
    W = WSlots(P, 3)
    sg = [P.sb("sg%d" % i, [128, 512]) for i in range(2)]
    pT = P.sb("pT", [128, 2, 512], F32R)
    ptok = P.sb("ptok", [128, 4, 256])
    G = P.sb("G", [128, 4, 16])
    rs = P.sb("rs", [128, 96])
    stat = P.sb("stat", [128, 8])
    epsc = P.sb("epsc", [128, 2])
    op("dve", I("memset", epsc[:, 0:1], LN_EPS), writes=["epsc"])
    pb = [P.ps("pb%d" % i, [128, 512]) for i in range(8)]
    Akeys = ["A%d" % tt for tt in range(4)]
    Bk = lambda tt: "B%d" % tt

    for st in range(NST):
        t0 = st * 512
        for q4 in range(4):
            op("pool", I("dma_start", out=bufA[:, q4 * 4:(q4 + 1) * 4, :], in_=mixT[q4 * 512:(q4 + 1) * 512, t0:t0 + 512].rearrange("(c p) t -> p c t", p=128)),
               writes=Akeys, dma="bufA")
        for nb in range(8):
            wt, wk = W.load([(lambda t: t[:, :].rearrange("p (c n) -> p c n", c=16),
                              w_out[:, nb * 256:(nb + 1) * 256].rearrange("(c p) n -> p c n", p=128))])
            wv = wt[:, :].rearrange("p (c n) -> p c n", c=16)
            for tt in range(4):
                bank, hq = tt, 0
                for c in range(16):
                    op("pe", I("matmul", pb[bank][:, hq * 128:hq * 128 + 256], lhsT=bufA[:, c, tt * 128:(tt + 1) * 128], rhs=wv[:, c, :], start=(c == 0), stop=(c == 15)),
                       reads=[wk, Akeys[tt]], writes=[pq(bank, hq), pq(bank, hq + 1)])
                op("act", I("activation", out=bufB[:, tt, nb * 256:(nb + 1) * 256], in_=pb[bank][:, hq * 128:hq * 128 + 256], func=AF.Copy),
                   reads=[pq(bank, hq), pq(bank, hq + 1)], writes=[Bk(tt)])
        op("sp", I("dma_start", out=lnb[:, 0, :], in_=ln1[0:1, :].partition_broadcast(128)), writes=["lnb"], dma="lnb")
        op("sp", I("dma_start", out=lnb[:, 1, :], in_=ln1[1:2, :].partition_broadcast(128)), writes=["lnb"], dma="lnb")
        for tt in range(4):
            op("sp", I("dma_start", out=xt[:], in_=x[t0 + tt * 128:t0 + (tt + 1) * 128, :]), writes=["xt"], dma="xt")
            op("dve", I("scalar_tensor_tensor", out=bufB[:, tt, :], in0=xt[:], scalar=ALPHA, in1=bufB[:, tt, :], op0=ALU.mult, op1=ALU.add),
               reads=["xt", Bk(tt)], writes=[Bk(tt)])
            layer_norm_tile(P, bufB[:, tt, :], Bk(tt), tmpC, lnb, epsc, stat)
            for cg in range(4):
                bank = 6 + cg % 2
                for c4 in range(4):
                    c = cg * 4 + c4
                    op("pe", I("transpose", pb[bank][:, c4 * 128:(c4 + 1) * 128], tmpC[:, c * 128:(c + 1) * 128], ident[:]),
                       reads=["tmpC", "ident"], writes=[pq(bank, c4)])
                if cg % 2 == 0:
                    op("act", I("activation", out=bufA[:, cg * 4:(cg + 1) * 4, tt * 128:(tt + 1) * 128], in_=pb[bank][:, :].rearrange("p (c n) -> p c n", c=4), func=AF.Copy),
                       reads=pbank(bank), writes=[Akeys[tt]])
                else:
                    op("dve", I("tensor_copy", out=bufA[:, cg * 4:(cg + 1) * 4, tt * 128:(tt + 1) * 128], in_=pb[bank][:, :].rearrange("p (c n) -> p c n", c=4)),
                       reads=pbank(bank), writes=[Akeys[tt]])
            op("act", I("activation", out=bufB[:, tt, :], in_=tmpC[:], func=AF.Copy, scale=ALPHA), reads=["tmpC"], writes=[Bk(tt)])
            if DBG and st == 0 and tt == 0:
                op("sp", I("dma_start", out=dbg[0], in_=tmpC[:]), reads=["tmpC"], writes=["dbg"], dma="dbg")
            for c in range(16):
                op("pe", I("matmul", pb[5][:, 0:16], lhsT=bufA[:, c, tt * 128:(tt + 1) * 128], rhs=wr[:, c, :], start=(c == 0), stop=(c == 15)),
                   reads=[Akeys[tt], "wr"], writes=pbank(5))
            lg = rs[:, 0:16]
            op("dve", I("tensor_tensor", out=lg, in0=pb[5][:, 0:16], in1=brt[:], op=ALU.add), reads=pbank(5) + ["brt"], writes=["rs"])
            op("dve", I("tensor_reduce", out=rs[:, 16:17], in_=lg, axis=AX.X, op=ALU.max), reads=["rs"], writes=["rs"])
            op("dve", I("tensor_scalar", out=lg, in0=lg, scalar1=rs[:, 16:17], scalar2=None, op0=ALU.subtract), reads=["rs"], writes=["rs"])
            op("act", I("activation", out=lg, in_=lg, func=AF.Exp), reads=["rs"], writes=["rs"])
            pr3 = rs[:, 0:16].rearrange("p (g e) -> p g e", g=4)
            m1 = rs[:, 20:24]
            m2 = rs[:, 24:28]
            gsc = rs[:, 28:32]
            msk = rs[:, 32:48]
            msk3 = rs[:, 32:48].rearrange("p (g e) -> p g e", g=4)
            op("dve", I("tensor_reduce", out=m1, in_=pr3, axis=AX.X, op=ALU.max), reads=["rs"], writes=["rs"])
            op("dve", I("tensor_tensor", out=msk3, in0=pr3, in1=m1.unsqueeze(2).to_broadcast([128, 4, 4]), op=ALU.is_lt), reads=["rs"], writes=["rs"])
            op("dve", I("tensor_tensor", out=msk, in0=msk, in1=rs[:, 0:16], op=ALU.mult), reads=["rs"], writes=["rs"])
            op("dve", I("tensor_reduce", out=m2, in_=msk3, axis=AX.X, op=ALU.max), reads=["rs"], writes=["rs"])
            op("dve", I("tensor_tensor", out=gsc, in0=m1, in1=m2, op=ALU.add), reads=["rs"], writes=["rs"])
            op("dve", I("tensor_reduce", out=rs[:, 48:49], in_=gsc, axis=AX.X, op=ALU.max), reads=["rs"], writes=["rs"])
            op("dve", I("tensor_scalar", out=rs[:, 52:56], in0=gsc, scalar1=rs[:, 48:49], scalar2=None, op0=ALU.is_ge), reads=["rs"], writes=["rs"])
            op("dve", I("tensor_tensor", out=msk3, in0=pr3, in1=m2.unsqueeze(2).to_broadcast([128, 4, 4]), op=ALU.is_ge), reads=["rs"], writes=["rs"])
            op("dve", I("tensor_tensor", out=msk3, in0=msk3, in1=rs[:, 52:56].unsqueeze(2).to_broadcast([128, 4, 4]), op=ALU.mult), reads=["rs"], writes=["rs"])
            op("dve", I("tensor_tensor", out=msk, in0=msk, in1=rs[:, 0:16], op=ALU.mult), reads=["rs"], writes=["rs"])
            op("dve", I("reciprocal", out=rs[:, 49:50], in_=rs[:, 48:49]), reads=["rs"], writes=["rs"])
            op("dve", I("tensor_scalar", out=G[:, tt, :], in0=msk, scalar1=rs[:, 49:50], scalar2=None, op0=ALU.mult), reads=["rs"], writes=["G"])
        for e_ in range(NEXP):
            for hbk in range(8):
                wt, wk = W.load([
                    (lambda t: t[:, 0:2048].rearrange("p (c n) -> p c n", c=16), w_eg[e_, :, hbk * 128:(hbk + 1) * 128].rearrange("(c p) n -> p c n", p=128)),
                    (lambda t: t[:, 2048:4096].rearrange("p (c n) -> p c n", c=16), w_eu[e_, :, hbk * 128:(hbk + 1) * 128].rearrange("(c p) n -> p c n", p=128)),
                ])
                wg = wt[:, 0:2048].rearrange("p (c n) -> p c n", c=16)
                wu = wt[:, 2048:4096].rearrange("p (c n) -> p c n", c=16)
                bg, bu = (hbk % 2) * 2, (hbk % 2) * 2 + 1
                for c in range(16):
                    op("pe", I("matmul", pb[bg][:, :], lhsT=wg[:, c, :], rhs=bufA[:, c, :], start=(c == 0), stop=(c == 15)), reads=[wk] + Akeys, writes=pbank(bg))
                for c in range(16):
                    op("pe", I("matmul", pb[bu][:, :], lhsT=wu[:, c, :], rhs=bufA[:, c, :], start=(c == 0), stop=(c == 15)), reads=[wk] + Akeys, writes=pbank(bu))
                sgt, sgk = sg[hbk % 2], "sg%d" % (hbk % 2)
                op("act", I("activation", out=sgt[:], in_=pb[bg][:, :], func=AF.Silu), reads=pbank(bg), writes=[sgk])
                op("dve", I("tensor_tensor", out=hid[:, hbk, :], in0=pb[bu][:, :], in1=sgt[:], op=ALU.mult), reads=pbank(bu) + [sgk], writes=["hid%d" % hbk])
            for nb in range(4):
                wt, wk = W.load([(lambda t: t[:, :].rearrange("p (c n) -> p c n", c=8),
                                  w_ed[e_, :, nb * 512:(nb + 1) * 512].rearrange("(c p) n -> p c n", p=128))])
                wd = wt[:, :].rearrange("p (c n) -> p c n", c=8)
                for tt in range(4):
                    bank = 4 + (nb * 4 + tt) % 2
                    for hbk in range(8):
                        op("pe", I("matmul", pb[bank][:, :], lhsT=hid[:, hbk, tt * 128:(tt + 1) * 128], rhs=wd[:, hbk, :], start=(hbk == 0), stop=(hbk == 7)),
                           reads=[wk, "hid%d" % hbk], writes=pbank(bank))
                    op("dve", I("scalar_tensor_tensor", out=bufB[:, tt, nb * 512:(nb + 1) * 512], in0=pb[bank][:, :], scalar=G[:, tt, e_:e_ + 1], in1=bufB[:, tt, nb * 512:(nb + 1) * 512], op0=ALU.mult, op1=ALU.add),
                       reads=pbank(bank) + ["G", Bk(tt)], writes=[Bk(tt)])
        if DBG and st == 0:
            op("sp", I("dma_start", out=dbg[1], in_=bufB[:, 0, :]), reads=[Bk(0)], writes=["dbg"], dma="dbg")
            op("sp", I("dma_start", out=dbg[2, :, 0:64], in_=G[:].rearrange("p a b -> p (a b)")), reads=["G"], writes=["dbg"], dma="dbg")
        op("sp", I("dma_start", out=lnb[:, 0, :], in_=ln2[0:1, :].partition_broadcast(128)), writes=["lnb"], dma="lnb")
        op("sp", I("dma_start", out=lnb[:, 1, :], in_=ln2[1:2, :].partition_broadcast(128)), writes=["lnb"], dma="lnb")
        op("sp", I("dma_start", out=ptok[:], in_=p[t0:t0 + 512, :].rearrange("(tt p) n -> p tt n", p=128)), writes=["ptok"], dma="ptok")
        for tt in range(4):
            layer_norm_tile(P, bufB[:, tt, :], Bk(tt), tmpC, lnb, epsc, stat)
            for cg in range(4):
                bank = 6 + cg % 2
                for c4 in range(4):
                    c = cg * 4 + c4
                    op("pe", I("transpose", pb[bank][:, c4 * 128:(c4 + 1) * 128], tmpC[:, c * 128:(c + 1) * 128], ident[:]),
                       reads=["tmpC", "ident"], writes=[pq(bank, c4)])
                if cg % 2 == 0:
                    op("act", I("activation", out=bufA[:, cg * 4:(cg + 1) * 4, tt * 128:(tt + 1) * 128], in_=pb[bank][:, :].rearrange("p (c n) -> p c n", c=4), func=AF.Copy),
                       reads=pbank(bank), writes=[Akeys[tt]])
                else:
                    op("dve", I("tensor_copy", out=bufA[:, cg * 4:(cg + 1) * 4, tt * 128:(tt + 1) * 128], in_=pb[bank][:, :].rearrange("p (c n) -> p c n", c=4)),
                       reads=pbank(bank), writes=[Akeys[tt]])
            op("act", I("activation", out=bufB[:, tt, :], in_=tmpC[:], func=AF.Copy), reads=["tmpC"], writes=[Bk(tt)])
            for c2 in range(2):
                op("pe", I("transpose", pb[5][:, c2 * 128:(c2 + 1) * 128], ptok[:, tt, c2 * 128:(c2 + 1) * 128], ident[:]), reads=["ptok", "ident"], writes=[pq(5, c2)])
            op("act", I("activation", out=pT[:, :, tt * 128:(tt + 1) * 128], in_=pb[5][:, 0:256].rearrange("p (c n) -> p c n", c=2), func=AF.Copy),
               reads=[pq(5, 0), pq(5, 1)], writes=["pT"])
        if DBG and st == 0:
            op("sp", I("dma_start", out=dbg[3], in_=bufB[:, 0, :]), reads=[Bk(0)], writes=["dbg"], dma="dbg")
        wpv, wpk = wppt[:], "wppt"
        for nb in range(8):
            wt, wk = W.load([(lambda t: t[:, :].rearrange("p (c n) -> p c n", c=16),
                              w_pg[:, nb * 256:(nb + 1) * 256].rearrange("(c p) n -> p c n", p=128))])
            wv = wt[:, :].rearrange("p (c n) -> p c n", c=16)
            for tt in range(4):
                bank = tt
                for c in range(16):
                    op("pe", I("matmul", pb[bank][:, 0:256], lhsT=bufA[:, c, tt * 128:(tt + 1) * 128], rhs=wv[:, c, :], start=(c == 0), stop=(c == 15)),
                       reads=[wk, Akeys[tt]], writes=[pq(bank, 0), pq(bank, 1)])
                for c2 in range(2):
                    op("pe", I("matmul", pb[bank][:, 256:512], lhsT=pT[:, c2, tt * 128:(tt + 1) * 128], rhs=wpv[:, c2, nb * 256:(nb + 1) * 256], start=(c2 == 0), stop=(c2 == 1)),
                       reads=[wpk, "pT"], writes=[pq(bank, 2), pq(bank, 3)])
                sgt, sgk = sg[tt % 2], "sg%d" % (tt % 2)
                op("act", I("activation", out=sgt[:, 0:256], in_=pb[bank][:, 0:256], func=AF.Sigmoid), reads=[pq(bank, 0), pq(bank, 1)], writes=[sgk])
                op("dve", I("tensor_tensor", out=sgt[:, 0:256], in0=pb[bank][:, 256:512], in1=sgt[:, 0:256], op=ALU.mult), reads=[pq(bank, 2), pq(bank, 3), sgk], writes=[sgk])
                op("dve", I("tensor_tensor", out=bufB[:, tt, nb * 256:(nb + 1) * 256], in0=bufB[:, tt, nb * 256:(nb + 1) * 256], in1=sgt[:, 0:256], op=ALU.add),
                   reads=[sgk, Bk(tt)], writes=[Bk(tt)])
        for tt in range(4):
            op("sp", I("dma_start", out=xo[t0 + tt * 128:t0 + (tt + 1) * 128, :], in_=bufB[:, tt, :]), reads=[Bk(tt)], writes=["xo%d" % tt], dma="xo%d" % tt)
    P.wait_all("sp", ["xo%d" % tt for tt in range(4)] + ["dbg"])
    P.emit()
    return nc


def _consts():
    idx = np.arange(128)
    same = (idx[:, None] // 64) == (idx[None, :] // 64)
    c = {}
    c["ident"] = np.eye(128, dtype=np.float32)
    c["Umat"] = (same & (idx[:, None] <= idx[None, :])).astype(np.float32)
    c["NEGmat"] = np.where(same & (idx[None, :] >= idx[:, None]), 0.0, -30000.0).astype(np.float32)
    c["STRmat"] = (same & (idx[None, :] > idx[:, None])).astype(np.float32)
    c["BDmat"] = same.astype(np.float32)
    c["cmask"] = np.stack([(idx // 64 == 0), (idx // 64 == 1)], 1).astype(np.float32)
    return c


def _bands(win):
    tp = np.arange(128)[:, None]
    t = np.arange(128)[None, :]
    cur = np.where((tp <= t) & (tp > t - win), 1.0 / win, 0.0) - (tp == t)
    prev = np.where((tp - 128 > t - win), 1.0 / win, 0.0)
    cnt = np.minimum(t + 1, win).astype(np.float64)
    cur0 = np.where((tp <= t) & (tp > t - win), 1.0 / cnt, 0.0) - (tp == t)
    return cur.astype(np.float32), prev.astype(np.float32), cur0.astype(np.float32)


def stage_a_inputs(i, fh, xb, w_in, conv_w, a_log, dt_bias, dn_norm_w, pool_w, pool_scale):
    heads = [4 * fh + hl for hl in range(4)]
    cols = []
    for h in heads:
        for kind in range(4):
            cols.append(np.arange(kind * 1024 + h * 128, kind * 1024 + (h + 1) * 128))
    cols = np.concatenate(cols)
    m = {"x": np.ascontiguousarray(xb)}
    m["wA"] = np.ascontiguousarray(w_in[i][:, cols])
    ucols = np.concatenate([np.arange(4112 + (2 * fh + g) * 256, 4112 + (2 * fh + g + 1) * 256) for g in range(2)])
    m["wU"] = np.ascontiguousarray(w_in[i][:, ucols])
    bac = np.array([4096 + h for h in heads] + [4104 + h for h in heads])
    m["wba"] = np.ascontiguousarray(w_in[i][:, bac])
    cw = np.zeros((128, 12, 4), np.float32)
    for hl, h in enumerate(heads):
        for kind in range(3):
            cw[:, hl * 3 + kind, :] = conv_w[i][:, kind * 1024 + h * 128:kind * 1024 + (h + 1) * 128].T
    m["convw"] = cw
    m["negA"] = np.ascontiguousarray(np.broadcast_to(a_log[i][heads][None, :], (128, 4))).astype(np.float32)
    m["dtb"] = np.ascontiguousarray(np.broadcast_to(dt_bias[i][heads][None, :], (128, 4))).astype(np.float32)
    m["normw"] = np.ascontiguousarray(dn_norm_w[i][:, None]).astype(np.float32)
    m["poolw"] = np.ascontiguousarray(pool_w[i][2 * fh:2 * fh + 2])
    ps = np.zeros((128, 4), np.float32)
    for g in range(2):
        for oc in range(2):
            ps[:, g * 2 + oc] = pool_scale[i][(2 * fh + g) * 256 + oc * 128:(2 * fh + g) * 256 + (oc + 1) * 128]
    m["pscale"] = ps
    bc, bp, b0 = [], [], []
    for g in range(2):
        a, b, c = _bands(POOL_WINDOWS[2 * fh + g])
        bc.append(a); bp.append(b); b0.append(c)
    m["bcur"] = np.stack(bc); m["bprev"] = np.stack(bp); m["bcur0"] = np.stack(b0)
    m.update(_consts())
    return m


def stage_b_inputs(i, mixT_b, xb, pb_, w_out, ln1_g, ln1_b, w_router, b_router, w_e_gate, w_e_up, w_e_down,
                   ln2_g, ln2_b, w_ple_proj, w_ple_gate):
    m = {"mixT": np.ascontiguousarray(mixT_b), "x": np.ascontiguousarray(xb), "p": np.ascontiguousarray(pb_)}
    m["w_out"] = w_out[i]
    m["ln1"] = np.stack([ln1_g[i], ln1_b[i]])
    m["ln2"] = np.stack([ln2_g[i], ln2_b[i]])
    m["w_router"] = w_router
    m["b_router"] = np.ascontiguousarray(b_router[None, :])
    m["w_eg"] = w_e_gate[i]
    m["w_eu"] = w_e_up[i]
    m["w_ed"] = w_e_down[i]
    m["w_pp"] = w_ple_proj[i]
    m["w_pg"] = w_ple_gate[i]
    m["ident"] = np.eye(128, dtype=np.float32)
    return m


_NC_CACHE = {}


def _get_nc(kind, n):
    key = (kind, n)
    if key not in _NC_CACHE:
        _NC_CACHE[key] = build_stage_a(n) if kind == "a" else build_stage_b(n)
    return _NC_CACHE[key]


def kernel(x, p, w_in, conv_w, a_log, dt_bias, dn_norm_w, pool_w, pool_scale, w_out,
           ln1_g, ln1_b, w_router, b_router, w_e_gate, w_e_up, w_e_down, ln2_g, ln2_b,
           w_ple_proj, w_ple_gate):
    f = lambda a: np.asarray(a, dtype=np.float32)
    (x, p, w_in, conv_w, a_log, dt_bias, dn_norm_w, pool_w, pool_scale, w_out, ln1_g, ln1_b, w_router, b_router,
     w_e_gate, w_e_up, w_e_down, ln2_g, ln2_b, w_ple_proj, w_ple_gate) = map(f, (
        x, p, w_in, conv_w, a_log, dt_bias, dn_norm_w, pool_w, pool_scale, w_out, ln1_g, ln1_b, w_router, b_router,
        w_e_gate, w_e_up, w_e_down, ln2_g, ln2_b, w_ple_proj, w_ple_gate))
    B, S, _ = x.shape
    H = S // 2
    cur = x
    for i in range(DEPTH):
        nca = _get_nc("a", S)
        maps = [stage_a_inputs(i, c % 2, cur[c // 2], w_in, conv_w, a_log, dt_bias, dn_norm_w, pool_w, pool_scale)
                for c in range(8)]
        res = run_bass_kernel_spmd(nca, maps, core_ids=list(range(8)))
        mix = []
        for b in range(B):
            m0, m1 = res.results[2 * b]["mixT"], res.results[2 * b + 1]["mixT"]
            mix.append(np.concatenate([m0[0:512], m1[0:512], m0[512:1024], m1[512:1024]], 0))
        ncb = _get_nc("b", H)
        maps = []
        for c in range(8):
            b, th = c // 2, c % 2
            maps.append(stage_b_inputs(i, mix[b][:, th * H:(th + 1) * H], cur[b, th * H:(th + 1) * H], p[i, b, th * H:(th + 1) * H],
                                       w_out, ln1_g, ln1_b, w_router, b_router, w_e_gate, w_e_up, w_e_down,
                                       ln2_g, ln2_b, w_ple_proj, w_ple_gate))
        res = run_bass_kernel_spmd(ncb, maps, core_ids=list(range(8)))
        cur = np.stack([np.concatenate([res.results[2 * b]["xo"], res.results[2 * b + 1]["xo"]], 0) for b in range(B)])
    return cur.astype(np.float32)
```

```python
import contextlib
import os
LV = int(os.environ.get("LV", "9"))
import numpy as np
import concourse.bass as bass
import concourse.mybir as mybir
from concourse.bass_utils import run_bass_kernel_spmd

F32 = mybir.dt.float32
F32R = mybir.dt.float32r
AF = mybir.ActivationFunctionType
ALU = mybir.AluOpType
AX = mybir.AxisListType

D = 2048
DEPTH = 2
BATCH = 4
SEQ = 4096
NEXP = 16
DEXP = 1024
ALPHA = (2.0 * DEPTH) ** 0.25
LN_EPS = 1e-5
RMS_EPS = 1e-6
POOL_WINDOWS = (2, 4, 8, 16)


class Prog:
    ENGS = ("pe", "act", "dve", "pool", "sp")

    def __init__(self, nc):
        self.nc = nc
        self.outer = contextlib.ExitStack()
        self.stack = None
        self.pfx = ""
        self.ops = {e: [] for e in self.ENGS}
        self.cnt = {e: 0 for e in self.ENGS}
        self.esem = {}
        self.dsem = {}
        self.res = {}
        self.seen = {e: {} for e in self.ENGS}

    def begin_stage(self, pfx):
        self.pfx = pfx
        self.stack = contextlib.ExitStack()
        old = set(id(v) for v in self.esem.values())
        for key, ent in list(self.res.items()):
            if ent[0] is not None and id(ent[0][0]) in old:
                ent[0] = None
            for k in list(ent[1].keys()):
                if k in old:
                    del ent[1][k]
        for e in self.ENGS:
            self.esem[e] = self.outer.enter_context(self.nc.semaphore("sem_%s_%s" % (pfx, e)))
            self.cnt[e] = 0

    def end_stage(self):
        self.emit_block()
        self.stack.close()
        self.stack = None

    def sb(self, name, shape, dtype=F32):
        return self.stack.enter_context(self.nc.sbuf_tensor("s_" + self.pfx + name, list(shape), dtype))

    def ps(self, name, shape, dtype=F32):
        return self.stack.enter_context(self.nc.psum_tensor("p_" + self.pfx + name, list(shape), dtype))

    def _need(self, eng, tok, needs):
        if tok is None:
            return
        sem, val = tok
        if sem is self.esem.get(eng) and eng == "pe":
            return
        k = id(sem)
        if self.seen[eng].get(k, 0) >= val:
            return
        if k not in needs or needs[k][1] < val:
            needs[k] = (sem, val)

    enabled = True

    def op(self, eng, fn, reads=(), writes=(), dma=None, dinc=16):
        if not self.enabled:
            return None
        pk = set(k.split(".")[0] for k in list(reads) + list(writes) if k.startswith("pb"))
        reads = [k for k in reads if not k.startswith("pb")]
        writes = [k for k in writes if not k.startswith("pb")] + sorted(pk)
        needs = {}
        for r in reads:
            ent = self.res.get(r)
            if ent is not None:
                self._need(eng, ent[0], needs)
        for w in writes:
            ent = self.res.get(w)
            if ent is not None:
                self._need(eng, ent[0], needs)
                for tok in ent[1].values():
                    self._need(eng, tok, needs)
        if dma is not None:
            if dma not in self.dsem:
                self.dsem[dma] = [self.outer.enter_context(self.nc.semaphore("dq_" + dma)), 0]
            ds = self.dsem[dma]
            if ds[1] > 0:
                self._need(eng, (ds[0], ds[1]), needs)
            ds[1] += dinc
            tok = (ds[0], ds[1])
            inc = (ds[0], dinc)
        else:
            self.cnt[eng] += 1
            tok = (self.esem[eng], self.cnt[eng])
            inc = (self.esem[eng], 1)
        waits = list(needs.values())
        for sem, val in waits:
            self.seen[eng][id(sem)] = val
        self.ops[eng].append((waits, fn, inc))
        for r in reads:
            ent = self.res.setdefault(r, [None, {}])
            k = id(tok[0])
            if k not in ent[1] or ent[1][k][1] < tok[1]:
                ent[1][k] = tok
        for w in writes:
            self.res[w] = [tok, {}]
        return tok

    def wait_all(self, eng, keys):
        needs = {}
        for k in keys:
            ent = self.res.get(k)
            if ent is not None:
                self._need(eng, ent[0], needs)
        self.ops[eng].append((list(needs.values()), None, None))

    def emit_block(self):
        nc = self.nc
        engmap = {"pe": "tensor", "act": "scalar", "dve": "vector", "pool": "gpsimd", "sp": "sync"}
        with nc.Block() as block:
            for e in self.ENGS:
                lst = self.ops[e]

                def body(eng, lst=lst):
                    for waits, fn, inc in lst:
                        for sem, val in waits:
                            eng.wait_ge(sem, val)
                        if fn is not None:
                            ins = fn(eng)
                            if inc[1] == 1 and inc[0] not in self.esem.values():
                                ins.then_inc(inc[0])
                            else:
                                ins.then_inc(inc[0], inc[1])

                getattr(block, engmap[e])(body)
        self.ops = {e: [] for e in self.ENGS}

    def finish(self):
        self.outer.close()


class WSlots:
    def __init__(self, P, n, name="W", size=4096):
        self.P = P
        self.n = n
        self.name = name
        self.t = [P.sb("%s%d" % (name, i), [128, size], F32R) for i in range(n)]
        self.i = 0

    def load(self, parts):
        s = self.i % self.n
        self.i += 1
        key = "%s%d" % (self.name, s)
        t = self.t[s]
        for dv, src in parts:
            self.P.op("pool", I("dma_start", out=dv(t), in_=src),
                      writes=[key], dma=key)
        return t, key


def I(m, *a, **k):
    return lambda e: getattr(e, m)(*a, **k)


def pq(b, q):
    return "pb%d.%d" % (b, q)


def pbank(b):
    return [pq(b, q) for q in range(4)]


_SHARED = {}


def _sh(nc, name, shape):
    key = (id(nc), name)
    if key not in _SHARED:
        _SHARED[key] = nc.dram_tensor("c_" + name, list(shape), F32, kind="ExternalInput").ap()
    return _SHARED[key]


def stage_a(nc, P, NT, pfx, x, xkey, MA):
    NST = NT // 512
    HST = NST // 2
    dr = lambda n, s, k="ExternalInput": nc.dram_tensor(pfx + n, list(s), F32, kind=k).ap()
    wA = dr("wA", [D, 2048])
    wU = dr("wU", [D, 512])
    wba = dr("wba", [D, 8])
    convw = dr("convw", [128, 12, 4])
    negA_d = dr("negA", [128, 4])
    dtb_d = dr("dtb", [128, 4])
    normw_d = dr("normw", [128, 1])
    poolw_d = dr("poolw", [2, 256, 256])
    pscale_d = dr("pscale", [128, 4])
    bcur_d = _sh(nc, "bcur", [2, 128, 128])
    bprev_d = _sh(nc, "bprev", [2, 128, 128])
    bcur0_d = _sh(nc, "bcur0", [2, 128, 128])
    ident_d = _sh(nc, "ident", [128, 128])
    U_d = _sh(nc, "Umat", [128, 128])
    NEG_d = _sh(nc, "NEGmat", [128, 128])
    STR_d = _sh(nc, "STRmat", [128, 128])
    BD_d = _sh(nc, "BDmat", [128, 128])
    cm_d = _sh(nc, "cmask", [128, 2])
    DBG = 0
    P.begin_stage(pfx)
    op = P.op
    consts = {}

    def cload(name, src, shape, dtype=F32, q="sp"):
        t = P.sb(name, shape, dtype)
        if len(shape) == 2:
            op(q, I("dma_start", out=t[:], in_=src), writes=[name], dma="const")
        else:
            op(q, I("dma_start", out=t[:], in_=src), writes=[name], dma="const")
        consts[name] = t
        return t

    ident = cload("ident", ident_d, [128, 128])
    Um = cload("Um", U_d, [128, 128])
    NEGm = cload("NEGm", NEG_d, [128, 128])
    STRm = cload("STRm", STR_d, [128, 128])
    BDm = cload("BDm", BD_d, [128, 128])
    cmk = cload("cmk", cm_d, [128, 2])
    cw = cload("cw", convw, [128, 12, 4])
    alog = cload("alog", negA_d, [128, 4])
    dtb = cload("dtb", dtb_d, [128, 4])
    normw = cload("normw", normw_d, [128, 1])
    pscale = cload("pscale", pscale_d, [128, 4])
    bcur = cload("bcur", bcur_d.rearrange("g p n -> p g n"), [128, 2, 128])
    bprev = cload("bprev", bprev_d.rearrange("g p n -> p g n"), [128, 2, 128])
    bcur0 = cload("bcur0", bcur0_d.rearrange("g p n -> p g n"), [128, 2, 128])
    wbat = cload("wbat", wba.rearrange("(c p) n -> p c n", p=128), [128, 16, 8], F32R, q="pool")
    poolw = cload("poolw", poolw_d.rearrange("g (c p) n -> p g c n", p=128), [128, 2, 2, 256], F32R, q="pool")
    ones = P.sb("ones", [128, 128])
    op("dve", I("memset", ones[:], 1.0), writes=["ones"])
    epsc = P.sb("epsc", [128, 2])
    op("dve", I("memset", epsc[:, 0:1], RMS_EPS), writes=["epsc"])
    negA = P.sb("negA", [128, 4])
    op("act", I("activation", out=negA[:], in_=alog[:], func=AF.Exp), reads=["alog"], writes=["negA"])
    op("dve", I("tensor_scalar", out=negA[:], in0=negA[:], scalar1=-1.0, scalar2=None, op0=ALU.mult),
       reads=["negA"], writes=["negA"])

    xtok = [P.sb("xtok%d" % i, [128, D]) for i in range(2)]
    xT = P.sb("xT", [128, 16, 512], F32R)
    W = WSlots(P, 3, size=2048)
    raw = P.sb("raw", [128, 12, 515])
    cq = P.sb("cq", [128, 12, 512])
    szT = P.sb("szT", [128, 4, 512])
    ut = P.sb("utok", [128, 4, 512])
    utk = "utok"
    uprev = P.sb("uprev", [128, 512])
    dT = P.sb("dT", [128, 4, 512], F32R)
    poolst = P.sb("poolst", [128, 4, 512])
    oT = P.sb("oT", [128, 4, 512])
    tmpa = P.sb("tmpa", [128, 512])
    tmpb = P.sb("tmpb", [128, 512])
    S = [P.sb("S%d" % h, [128, 128]) for h in range(4)]
    sms = [P.sb("sm%d" % i, [128, 64]) for i in range(2)]
    PREP = ["gU", "E", "Es", "kg", "vtok", "X0", "X1", "Y0", "Y1", "R0", "R1", "tD"]
    REC = ["qdec", "aqk", "kdec", "u0b", "w0t", "vnew"]
    hbt = {}
    for nm in PREP:
        for hp in range(2):
            hbt[(nm, hp)] = P.sb("%s_%d" % (nm, hp), [128, 128])
    for nm in REC:
        for h in range(4):
            hbt[(nm, h)] = P.sb("%s_%d" % (nm, h), [128, 128])

    def HT(nm, h):
        return hbt[(nm, h % 2)] if nm in PREP else hbt[(nm, h)]

    def HK(nm, h):
        return "%s_%d" % (nm, h % 2) if nm in PREP else "%s_%d" % (nm, h)

    pb = [P.ps("pb%d" % i, [128, 512]) for i in range(8)]
    HB = [0, 1, 4, 7]

    for h in range(4):
        op("dve", I("memset", S[h][:], 0.0), writes=["S%d" % h])
    op("dve", I("memset", raw[:], 0.0), writes=["raw%d" % bi for bi in range(12)])

    for st in range(NST):
        t0 = st * 512
        P.enabled = True
        for tt in range(4):
            xk = "xtok%d" % (tt % 2)
            xtt = xtok[tt % 2]
            op("sp", I("dma_start", out=xtt[:], in_=x(t0 + tt * 128)), reads=([xkey] if xkey else []), writes=[xk], dma=xk)
            for cg in range(4):
                for c4 in range(4):
                    c = cg * 4 + c4
                    op("pe", I("transpose", pb[3][:, c4 * 128:(c4 + 1) * 128], xtt[:, c * 128:(c + 1) * 128], ident[:]),
                       reads=[xk, "ident"], writes=[pq(3, c4)])
                src = pb[3][:, :].rearrange("p (c n) -> p c n", c=4)
                dst = xT[:, cg * 4:(cg + 1) * 4, tt * 128:(tt + 1) * 128]
                if cg % 2 == 0:
                    op("act", I("activation", out=dst, in_=src, func=AF.Copy), reads=pbank(3), writes=["xT"])
                else:
                    op("dve", I("tensor_copy", out=dst, in_=src), reads=pbank(3), writes=["xT"])
        for blk in range(16):
            wt, wk = W.load([(lambda t: t[:, :].rearrange("p (c n) -> p c n", c=16),
                              wA[:, blk * 128:(blk + 1) * 128].rearrange("(c p) n -> p c n", p=128))])
            wv = wt[:, :].rearrange("p (c n) -> p c n", c=16)
            h, kind = blk // 4, blk % 4
            bank = blk % 2
            for c in range(16):
                op("pe", I("matmul", pb[bank][:, :], lhsT=wv[:, c, :], rhs=xT[:, c, :], start=(c == 0), stop=(c == 15)),
                   reads=[wk, "xT"], writes=pbank(bank))
            if kind < 3:
                bi = h * 3 + kind
                op("act", I("activation", out=raw[:, bi, 3:515], in_=pb[bank][:, :], func=AF.Copy),
                   reads=pbank(bank), writes=["raw%d" % bi])
            else:
                op("act", I("activation", out=szT[:, h, :], in_=pb[bank][:, :], func=AF.Silu),
                   reads=pbank(bank), writes=["szT"])
        if st > 0:
            op("dve", I("tensor_copy", out=uprev[:], in_=ut[:, 3, :]), reads=[utk], writes=["uprev"])
        for blk in range(4):
            wt, wk = W.load([(lambda t: t[:, :].rearrange("p (c n) -> p c n", c=16),
                              wU[:, blk * 128:(blk + 1) * 128].rearrange("(c p) n -> p c n", p=128))])
            wv = wt[:, :].rearrange("p (c n) -> p c n", c=16)
            for tt in range(4):
                for c in range(16):
                    op("pe", I("matmul", pb[2][:, tt * 128:(tt + 1) * 128], lhsT=xT[:, c, tt * 128:(tt + 1) * 128], rhs=wv[:, c, :], start=(c == 0), stop=(c == 15)),
                       reads=[wk, "xT"], writes=[pq(2, tt)])
            op("act", I("activation", out=ut[:, :, blk * 128:(blk + 1) * 128], in_=pb[2][:, :].rearrange("p (t n) -> p t n", t=4), func=AF.Copy),
               reads=pbank(2), writes=[utk])
        P.enabled = LV >= 2
        for bi in range(12):
            eng = "dve"
            rk = "raw%d" % bi
            op(eng, I("tensor_scalar", out=cq[:, bi, :], in0=raw[:, bi, 0:512], scalar1=cw[:, bi, 0:1], scalar2=None, op0=ALU.mult),
               reads=[rk, "cw"], writes=["cq%d" % bi])
            for j in range(1, 4):
                op(eng, I("scalar_tensor_tensor", out=cq[:, bi, :], in0=raw[:, bi, j:j + 512], scalar=cw[:, bi, j:j + 1], in1=cq[:, bi, :], op0=ALU.mult, op1=ALU.add),
                   reads=[rk, "cw"], writes=["cq%d" % bi])
            op("act", I("activation", out=cq[:, bi, :], in_=cq[:, bi, :], func=AF.Silu), reads=["cq%d" % bi], writes=["cq%d" % bi])
            op(eng, I("tensor_copy", out=raw[:, bi, 0:3], in_=raw[:, bi, 512:515]), reads=[rk], writes=[rk])
        for h in range(4):
            for kind in range(2):
                bi = h * 3 + kind
                ck = "cq%d" % bi
                op("act", I("activation", out=tmpa[:], in_=cq[:, bi, :], func=AF.Square), reads=[ck], writes=["tmpa"])
                op("pe", I("matmul", pb[2][:, :], lhsT=ones[:], rhs=tmpa[:], start=True, stop=True), reads=["ones", "tmpa"], writes=pbank(2))
                op("act", I("activation", out=tmpb[:], in_=pb[2][:, :], func=AF.Sqrt, bias=epsc[:, 0:1]), reads=pbank(2) + ["epsc"], writes=["tmpb"])
                op("dve", I("reciprocal", out=tmpb[:], in_=tmpb[:]), reads=["tmpb"], writes=["tmpb"])
                sc = (128.0 ** -0.5) if kind == 0 else 1.0
                op("dve", I("scalar_tensor_tensor", out=cq[:, bi, :], in0=cq[:, bi, :], scalar=sc, in1=tmpb[:], op0=ALU.mult, op1=ALU.mult),
                   reads=[ck, "tmpb"], writes=[ck])
        P.enabled = LV >= 3
        for tt in range(4):
            gt = st * 4 + tt
            for blk in range(4):
                g = blk // 2
                reg = pq(6, blk)
                dst = pb[6][:, blk * 128:(blk + 1) * 128]
                if gt == 0:
                    op("pe", I("matmul", dst, lhsT=ut[:, tt, blk * 128:(blk + 1) * 128], rhs=bcur0[:, g, :], start=True, stop=True),
                       reads=[utk, "bcur0"], writes=[reg])
                else:
                    if tt == 0:
                        op("pe", I("matmul", dst, lhsT=uprev[:, blk * 128:(blk + 1) * 128], rhs=bprev[:, g, :], start=True, stop=False),
                           reads=["uprev", "bprev"], writes=[reg])
                    else:
                        op("pe", I("matmul", dst, lhsT=ut[:, tt - 1, blk * 128:(blk + 1) * 128], rhs=bprev[:, g, :], start=True, stop=False),
                           reads=[utk, "bprev"], writes=[reg])
                    op("pe", I("matmul", dst, lhsT=ut[:, tt, blk * 128:(blk + 1) * 128], rhs=bcur[:, g, :], start=False, stop=True),
                       reads=[utk, "bcur"], writes=[reg])
            op("dve", I("tensor_copy", out=dT[:, :, tt * 128:(tt + 1) * 128], in_=pb[6][:, :].rearrange("p (b n) -> p b n", b=4)),
               reads=pbank(6), writes=["dT"])
        for g in range(2):
            for oc in range(2):
                blk = g * 2 + oc
                for cc in range(2):
                    op("pe", I("matmul", pb[5][:, :], lhsT=poolw[:, g, cc, oc * 128:(oc + 1) * 128], rhs=dT[:, g * 2 + cc, :], start=(cc == 0), stop=(cc == 1)),
                       reads=["poolw", "dT"], writes=pbank(5))
                op("dve", I("tensor_scalar", out=poolst[:, blk, :], in0=pb[5][:, :], scalar1=pscale[:, blk:blk + 1], scalar2=None, op0=ALU.mult),
                   reads=pbank(5) + ["pscale"], writes=["poolst"])
        mh, mc = st // HST, (st % HST) * 512
        for k2 in range(2):
            op("sp", I("dma_start", out=MA[mh][2 + k2][:, mc:mc + 512].rearrange("(b p) t -> p b t", p=128), in_=poolst[:, k2 * 2:(k2 + 1) * 2, :]),
               reads=["poolst"], writes=["MA%dp" % mh], dma="poolst")

        P.enabled = LV >= 4
        for tt in range(4):
            par = tt % 2
            c0 = tt * 128
            sm = sms[par]
            smk = "sm%d" % par
            for c in range(16):
                op("pe", I("matmul", pb[2][:, 0:8], lhsT=xT[:, c, c0:c0 + 128], rhs=wbat[:, c, :], start=(c == 0), stop=(c == 15)),
                   reads=["xT", "wbat"], writes=pbank(2))
            op("act", I("activation", out=sm[:, 0:4], in_=pb[2][:, 0:4], func=AF.Sigmoid), reads=pbank(2), writes=[smk])
            op("dve", I("tensor_scalar", out=sm[:, 4:8], in0=sm[:, 0:4], scalar1=-1.0, scalar2=None, op0=ALU.mult), reads=[smk], writes=[smk])
            op("dve", I("tensor_tensor", out=sm[:, 40:44], in0=pb[2][:, 4:8], in1=dtb[:], op=ALU.add), reads=pbank(2) + ["dtb"], writes=[smk])
            op("act", I("activation", out=sm[:, 40:44], in_=sm[:, 40:44], func=AF.Exp), reads=[smk], writes=[smk])
            op("act", I("activation", out=sm[:, 40:44], in_=sm[:, 40:44], func=AF.Ln, bias=1.0), reads=[smk], writes=[smk])
            op("dve", I("tensor_tensor", out=sm[:, 8:12], in0=sm[:, 40:44], in1=negA[:], op=ALU.mult), reads=[smk, "negA"], writes=[smk])
            for ch in range(2):
                op("dve", I("tensor_scalar", out=sm[:, 24 + ch * 4:28 + ch * 4], in0=sm[:, 8:12], scalar1=cmk[:, ch:ch + 1], scalar2=None, op0=ALU.mult),
                   reads=[smk, "cmk"], writes=[smk])
            op("pe", I("matmul", pb[2][:, 8:12], lhsT=Um[:], rhs=sm[:, 8:12], start=True, stop=True), reads=["Um", smk], writes=pbank(2))
            op("pe", I("matmul", pb[2][:, 12:16], lhsT=BDm[:], rhs=sm[:, 8:12], start=True, stop=True), reads=["BDm", smk], writes=pbank(2))
            op("pe", I("matmul", pb[2][:, 16:24], lhsT=ones[:], rhs=sm[:, 24:32], start=True, stop=True), reads=["ones", smk], writes=pbank(2))
            op("dve", I("tensor_copy", out=sm[:, 12:16], in_=pb[2][:, 8:12]), reads=pbank(2), writes=[smk])
            op("act", I("activation", out=sm[:, 16:20], in_=pb[2][:, 8:12], func=AF.Exp), reads=pbank(2), writes=[smk])
            op("dve", I("tensor_tensor", out=sm[:, 20:24], in0=pb[2][:, 12:16], in1=sm[:, 12:16], op=ALU.subtract), reads=pbank(2) + [smk], writes=[smk])
            op("act", I("activation", out=sm[:, 20:24], in_=sm[:, 20:24], func=AF.Exp), reads=[smk], writes=[smk])
            op("act", I("activation", out=sm[:, 32:40], in_=pb[2][:, 16:24], func=AF.Exp), reads=pbank(2), writes=[smk])

            P.enabled = LV >= 5
            for pair in range(2):
                heads = [pair * 2, pair * 2 + 1]
                for h in heads:
                    B = HB[h]
                    qTv = cq[:, h * 3 + 0, c0:c0 + 128]
                    kTv = cq[:, h * 3 + 1, c0:c0 + 128]
                    vTv = cq[:, h * 3 + 2, c0:c0 + 128]
                    qk_, kk_, vk_ = "cq%d" % (h * 3), "cq%d" % (h * 3 + 1), "cq%d" % (h * 3 + 2)
                    T = lambda nm, h=h: HT(nm, h)
                    K = lambda nm, h=h: HK(nm, h)
                    op("pe", I("transpose", pb[B][:, 0:128], kTv, ident[:]), reads=[kk_, "ident"], writes=[pq(B, 0)])
                    op("pe", I("transpose", pb[B][:, 128:256], vTv, ident[:]), reads=[vk_, "ident"], writes=[pq(B, 1)])
                    op("dve", I("tensor_scalar", out=T("kg")[:], in0=pb[B][:, 0:128], scalar1=sm[:, 16 + h:17 + h], scalar2=None, op0=ALU.mult),
                       reads=[pq(B, 0), smk], writes=[K("kg")])
                    op("dve", I("tensor_scalar", out=T("kdec")[:], in0=pb[B][:, 0:128], scalar1=sm[:, 20 + h:21 + h], scalar2=None, op0=ALU.mult),
                       reads=[pq(B, 0), smk], writes=[K("kdec")])
                    op("act", I("activation", out=T("vtok")[:], in_=pb[B][:, 128:256], func=AF.Copy), reads=[pq(B, 1)], writes=[K("vtok")])
                    op("dve", I("tensor_scalar", out=T("gU")[:], in0=Um[:], scalar1=sm[:, 8 + h:9 + h], scalar2=None, op0=ALU.mult),
                       reads=["Um", smk], writes=[K("gU")])
                    op("pe", I("matmul", pb[B][:, 256:384], lhsT=ones[:], rhs=T("gU")[:], start=True, stop=True), reads=["ones", K("gU")], writes=[pq(B, 2)])
                    op("dve", I("scalar_tensor_tensor", out=T("tD")[:], in0=pb[B][:, 256:384], scalar=sm[:, 12 + h:13 + h], in1=NEGm[:], op0=ALU.subtract, op1=ALU.add),
                       reads=[pq(B, 2), smk, "NEGm"], writes=[K("tD")])
                    op("act", I("activation", out=T("E")[:], in_=T("tD")[:], func=AF.Exp), reads=[K("tD")], writes=[K("E")])
                    op("act", I("activation", out=T("tD")[:], in_=pb[B][:, 256:384], func=AF.Exp), reads=[pq(B, 2), K("E")], writes=[K("tD")])
                    op("pool", I("tensor_tensor", out=T("qdec")[:], in0=qTv, in1=T("tD")[:], op=ALU.mult),
                       reads=[qk_, K("tD")], writes=[K("qdec")])
                    op("pool", I("tensor_tensor", out=T("Es")[:], in0=T("E")[:], in1=STRm[:], op=ALU.mult), reads=[K("E"), "STRm"], writes=[K("Es")])
                    op("pe", I("matmul", pb[B][:, 0:128], lhsT=kTv, rhs=kTv, start=True, stop=True), reads=[kk_], writes=[pq(B, 0)])
                    op("pe", I("matmul", pb[B][:, 128:256], lhsT=kTv, rhs=qTv, start=True, stop=True), reads=[kk_, qk_], writes=[pq(B, 1)])
                    op("dve", I("tensor_tensor", out=T("aqk")[:], in0=pb[B][:, 128:256], in1=T("E")[:], op=ALU.mult),
                       reads=[pq(B, 1), K("E")], writes=[K("aqk")])
                    op("dve", I("scalar_tensor_tensor", out=T("X0")[:], in0=pb[B][:, 0:128], scalar=sm[:, 4 + h:5 + h], in1=T("Es")[:], op0=ALU.mult, op1=ALU.mult),
                       reads=[pq(B, 0), smk, K("Es")], writes=[K("X0")])
                    op("pe", I("transpose", pb[B][:, 384:512], T("X0")[:], ident[:]), reads=[K("X0"), "ident"], writes=[pq(B, 3)])
                    op("act", I("activation", out=T("Y0")[:], in_=pb[B][:, 384:512], func=AF.Copy), reads=[pq(B, 3)], writes=[K("Y0")])
                    op("pool", I("tensor_tensor", out=T("R0")[:], in0=T("X0")[:], in1=ident[:], op=ALU.add), reads=[K("X0"), "ident"], writes=[K("R0")])
                for lvl in range(5):
                    a, b = lvl % 2, (lvl + 1) % 2
                    for h in heads:
                        B = HB[h]
                        Xa, Ya, Xb, Yb = HT("X%d" % a, h), HT("Y%d" % a, h), HT("X%d" % b, h), HT("Y%d" % b, h)
                        kXa, kYa, kXb, kYb = HK("X%d" % a, h), HK("Y%d" % a, h), HK("X%d" % b, h), HK("Y%d" % b, h)
                        if lvl < 4:
                            op("pe", I("matmul", pb[B][:, 0:128], lhsT=Ya[:], rhs=Xa[:], start=True, stop=True), reads=[kXa, kYa], writes=[pq(B, 0)])
                        op("pe", I("matmul", pb[B][:, 128:256], lhsT=Xa[:], rhs=Ya[:], start=True, stop=True), reads=[kXa, kYa], writes=[pq(B, 1)])
                        if lvl < 4:
                            op("act", I("activation", out=Xb[:], in_=pb[B][:, 0:128], func=AF.Copy), reads=[pq(B, 0)], writes=[kXb])
                        op("dve", I("tensor_copy", out=Yb[:], in_=pb[B][:, 128:256]), reads=[pq(B, 1)], writes=[kYb])
                    for h in heads:
                        B = HB[h]
                        Yb = HT("Y%d" % b, h)
                        Ra, Rb = HT("R%d" % a, h), HT("R%d" % b, h)
                        kYb, kRa, kRb = HK("Y%d" % b, h), HK("R%d" % a, h), HK("R%d" % b, h)
                        op("pe", I("matmul", pb[B][:, 256:384], lhsT=Yb[:], rhs=Ra[:], start=True, stop=True), reads=[kYb, kRa], writes=[pq(B, 2)])
                        op("dve", I("tensor_tensor", out=Rb[:], in0=pb[B][:, 256:384], in1=Ra[:], op=ALU.add), reads=[pq(B, 2), kRa], writes=[kRb])
                for h in heads:
                    B = HB[h]
                    R = HT("R1", h)
                    kR = HK("R1", h)
                    op("pe", I("matmul", pb[B][:, 384:512], lhsT=R[:], rhs=HT("vtok", h)[:], start=True, stop=True), reads=[kR, HK("vtok", h)], writes=[pq(B, 3)])
                    op("pe", I("matmul", pb[B][:, 0:128], lhsT=HT("kg", h)[:], rhs=R[:], start=True, stop=True), reads=[kR, HK("kg", h)], writes=[pq(B, 0)])
                    op("dve", I("tensor_scalar", out=HT("u0b", h)[:], in0=pb[B][:, 384:512], scalar1=sm[:, h:h + 1], scalar2=None, op0=ALU.mult),
                       reads=[pq(B, 3), smk], writes=[HK("u0b", h)])
                    op("act", I("activation", out=HT("w0t", h)[:], in_=pb[B][:, 0:128], func=AF.Copy), reads=[pq(B, 0)], writes=[HK("w0t", h)])
            P.enabled = LV >= 6
            for ch in range(2):
                r0 = ch * 64
                for h in range(4):
                    rb, rq = 5 + h // 2, (h % 2) * 2
                    op("pe", I("matmul", pb[rb][r0:r0 + 64, rq * 128:(rq + 1) * 128], lhsT=HT("w0t", h)[:, r0:r0 + 64], rhs=S[h][:], start=True, stop=True),
                       reads=[HK("w0t", h), "S%d" % h], writes=[pq(rb, rq)])
                for h in range(4):
                    rb, rq = 5 + h // 2, (h % 2) * 2
                    op("dve", I("scalar_tensor_tensor", out=HT("vnew", h)[r0:r0 + 64, :], in0=pb[rb][r0:r0 + 64, rq * 128:(rq + 1) * 128], scalar=sm[r0:r0 + 64, 4 + h:5 + h], in1=HT("u0b", h)[r0:r0 + 64, :], op0=ALU.mult, op1=ALU.add),
                       reads=[pq(rb, rq), smk, HK("u0b", h)], writes=[HK("vnew", h)])
                for h in range(4):
                    rb, rq = 5 + h // 2, (h % 2) * 2 + 1
                    ob = pq(3, h)
                    op("pe", I("matmul", pb[3][:, h * 128:h * 128 + 64], lhsT=S[h][:], rhs=HT("qdec", h)[:, r0:r0 + 64], start=True, stop=False),
                       reads=["S%d" % h, HK("qdec", h)], writes=[ob])
                    op("pe", I("matmul", pb[3][:, h * 128:h * 128 + 64], lhsT=HT("vnew", h)[r0:r0 + 64, :], rhs=HT("aqk", h)[r0:r0 + 64, r0:r0 + 64], start=False, stop=True),
                       reads=[HK("vnew", h), HK("aqk", h)], writes=[ob])
                    op("pe", I("matmul", pb[rb][:, rq * 128:(rq + 1) * 128], lhsT=HT("kdec", h)[r0:r0 + 64, :], rhs=HT("vnew", h)[r0:r0 + 64, :], start=True, stop=True),
                       reads=[HK("kdec", h), HK("vnew", h)], writes=[pq(rb, rq)])
                for h in range(4):
                    rb, rq = 5 + h // 2, (h % 2) * 2 + 1
                    op("act", I("activation", out=oT[:, h, c0 + r0:c0 + r0 + 64], in_=pb[3][:, h * 128:h * 128 + 64], func=AF.Copy),
                       reads=[pq(3, h)], writes=["oT"])
                    op("dve", I("scalar_tensor_tensor", out=S[h][:], in0=S[h][:], scalar=sm[:, 32 + ch * 4 + h:33 + ch * 4 + h], in1=pb[rb][:, rq * 128:(rq + 1) * 128], op0=ALU.mult, op1=ALU.add),
                       reads=["S%d" % h, smk, pq(rb, rq)], writes=["S%d" % h])
            if DBG and st == 0 and tt == 0:
                P.enabled = True
                items = [(cq[:, 0, 0:128], "cq0"), (cq[:, 1, 0:128], "cq1"), (cq[:, 2, 0:128], "cq2"), (sm[:, :], smk, 64),
                         (HT("E", 0)[:], HK("E", 0)), (HT("X0", 0)[:], HK("X0", 0)), (HT("R1", 0)[:], HK("R1", 0)),
                         (HT("u0b", 0)[:], HK("u0b", 0)), (HT("w0t", 0)[:], HK("w0t", 0)), (HT("qdec", 0)[:], HK("qdec", 0)),
                         (HT("aqk", 0)[:], HK("aqk", 0)), (HT("kdec", 0)[:], HK("kdec", 0)), (HT("vnew", 0)[:], HK("vnew", 0)),
                         (S[0][:], "S0"), (oT[:, 0, 0:128], "oT"), (HT("kg", 0)[:], HK("kg", 0)), (HT("vtok", 0)[:], HK("vtok", 0)),
                         (HT("Y0", 0)[:], HK("Y0", 0))]
                for di, it in enumerate(items):
                    w = it[2] if len(it) > 2 else 128
                    op("sp", I("dma_start", out=dbg[di, :, 0:w], in_=it[0]), reads=[it[1]], writes=["dbg"], dma="dbg")
        P.enabled = LV >= 7
        for h in range(4):
            op("act", I("activation", out=tmpa[:], in_=oT[:, h, :], func=AF.Square), reads=["oT"], writes=["tmpa"])
            op("pe", I("matmul", pb[2][:, :], lhsT=ones[:], rhs=tmpa[:], start=True, stop=True), reads=["ones", "tmpa"], writes=pbank(2))
            op("act", I("activation", out=tmpb[:], in_=pb[2][:, :], func=AF.Sqrt, bias=epsc[:, 0:1], scale=1.0 / 128.0), reads=pbank(2) + ["epsc"], writes=["tmpb"])
            op("dve", I("reciprocal", out=tmpb[:], in_=tmpb[:]), reads=["tmpb"], writes=["tmpb"])
            op("dve", I("scalar_tensor_tensor", out=tmpb[:], in0=oT[:, h, :], scalar=normw[:, 0:1], in1=tmpb[:], op0=ALU.mult, op1=ALU.mult),
               reads=["oT", "normw", "tmpb"], writes=["tmpb"])
            op("dve", I("tensor_tensor", out=oT[:, h, :], in0=tmpb[:], in1=szT[:, h, :], op=ALU.mult), reads=["tmpb", "szT"], writes=["oT"])
        P.enabled = True
        for k2 in range(2):
            op("sp", I("dma_start", out=MA[mh][k2][:, mc:mc + 512].rearrange("(b p) t -> p b t", p=128), in_=oT[:, k2 * 2:(k2 + 1) * 2, :]),
               reads=["oT"], writes=["MA%dd" % mh], dma="oT")
    P.wait_all("sp", ["MA0d", "MA0p", "MA1d", "MA1p"])


def layer_norm_tile(P, src, srck, tmpC, lnb, eps_t, stat):
    op = P.op
    op("dve", I("tensor_reduce", out=stat[:, 0:1], in_=src, axis=AX.X, op=ALU.add), reads=[srck], writes=["stat"])
    op("dve", I("tensor_scalar", out=stat[:, 1:2], in0=stat[:, 0:1], scalar1=1.0 / D, scalar2=None, op0=ALU.mult), reads=["stat"], writes=["stat"])
    op("dve", I("tensor_scalar", out=tmpC[:], in0=src, scalar1=stat[:, 1:2], scalar2=None, op0=ALU.subtract), reads=[srck, "stat"], writes=["tmpC"])
    op("act", I("activation", out=src, in_=tmpC[:], func=AF.Square), reads=["tmpC"], writes=[srck])
    op("dve", I("tensor_reduce", out=stat[:, 2:3], in_=src, axis=AX.X, op=ALU.add), reads=[srck], writes=["stat"])
    op("act", I("activation", out=stat[:, 3:4], in_=stat[:, 2:3], func=AF.Sqrt, bias=eps_t[:, 0:1], scale=1.0 / D), reads=["stat", "epsc"], writes=["stat"])
    op("dve", I("reciprocal", out=stat[:, 3:4], in_=stat[:, 3:4]), reads=["stat"], writes=["stat"])
    op("dve", I("scalar_tensor_tensor", out=tmpC[:], in0=tmpC[:], scalar=stat[:, 3:4], in1=lnb[:, 0, :], op0=ALU.mult, op1=ALU.mult),
       reads=["tmpC", "stat", "lnb"], writes=["tmpC"])
    op("dve", I("tensor_tensor", out=tmpC[:], in0=tmpC[:], in1=lnb[:, 1, :], op=ALU.add), reads=["tmpC", "lnb"], writes=["tmpC"])


def stage_b(nc, P, NTB, pfx, GM, sel_d, x, xkey, xo, xo_final):
    NST = NTB // 512
    dr = lambda n, s, k="ExternalInput": nc.dram_tensor(pfx + n, list(s), F32, kind=k).ap()
    p = dr("p", [NTB, 256])
    w_out = dr("w_out", [D, D])
    ln1 = dr("ln1", [2, D])
    ln2 = dr("ln2", [2, D])
    w_router = _sh(nc, "w_router", [D, 16])
    b_router = _sh(nc, "b_router", [1, 16])
    w_eg = dr("w_eg", [NEXP, D, DEXP])
    w_eu = dr("w_eu", [NEXP, D, DEXP])
    w_ed = dr("w_ed", [NEXP, DEXP, D])
    w_pp = dr("w_pp", [256, D])
    w_pg = dr("w_pg", [D, D])
    ident_d = _sh(nc, "ident", [128, 128])
    DBG = 0
    P.begin_stage(pfx)
    op = P.op
    sel = P.sb("sel", [128, 2])
    op("sp", I("dma_start", out=sel[:], in_=sel_d), writes=["sel"], dma="const")
    ident = P.sb("ident", [128, 128])
    op("sp", I("dma_start", out=ident[:], in_=ident_d), writes=["ident"], dma="const")
    wr = P.sb("wr", [128, 16, 16], F32R)
    op("pool", I("dma_start", out=wr[:], in_=w_router.rearrange("(c p) n -> p c n", p=128)), writes=["wr"], dma="const")
    brt = P.sb("brt", [128, 16])
    op("sp", I("dma_start", out=brt[:], in_=b_router.partition_broadcast(128)), writes=["brt"], dma="const")

    wppt = P.sb("wppt", [128, 2, D], F32R)
    op("pool", I("dma_start", out=wppt[:], in_=w_pp.rearrange("(c p) n -> p c n", p=128)), writes=["wppt"], dma="const")
    bufA = P.sb("bufA", [128, 16, 512], F32R)
    bufB = P.sb("bufB", [128, 4, D])
    xt = P.sb("xt", [128, D])
    tmpC = P.sb("tmpC", [128, D])
    lnb = P.sb("lnb", [128, 2, D])
    hid = P.sb("hid", [128, 8, 512], F32R)
    W = WSlots(P, 3)
    sg = [P.sb("sg%d" % i, [128, 512]) for i in range(2)]
    pT = P.sb("pT", [128, 2, 512], F32R)
    ptok = P.sb("ptok", [128, 4, 256])
    G = P.sb("G", [128, 4, 16])
    rs = P.sb("rs", [128, 96])
    stat = P.sb("stat", [128, 8])
    epsc = P.sb("epsc", [128, 2])
    op("dve", I("memset", epsc[:, 0:1], LN_EPS), writes=["epsc"])
    pb = [P.ps("pb%d" % i, [128, 512]) for i in range(8)]
    Akeys = ["A%d" % tt for tt in range(4)]
    Bk = lambda tt: "B%d" % tt

    for st in range(NST):
        t0 = st * 512
        for q4 in range(4):
            v0 = tmpC[:].rearrange("p (c t) -> p c t", c=4)
            v1 = xt[:].rearrange("p (c t) -> p c t", c=4)
            for k2 in range(2):
                j, lk = q4 // 2, (q4 % 2) * 2 + k2
                op("sp", I("dma_start", out=v0[:, k2 * 2:(k2 + 1) * 2, :], in_=GM[0][lk][j * 256:(j + 1) * 256, t0:t0 + 512].rearrange("(c p) t -> p c t", p=128)),
                   reads=["G0"], writes=["tmpC"], dma="tmpC")
                op("sp", I("dma_start", out=v1[:, k2 * 2:(k2 + 1) * 2, :], in_=GM[1][lk][j * 256:(j + 1) * 256, t0:t0 + 512].rearrange("(c p) t -> p c t", p=128)),
                   reads=["G1"], writes=["xt"], dma="xt")
            op("dve", I("tensor_scalar", out=tmpC[:], in0=tmpC[:], scalar1=sel[:, 0:1], scalar2=None, op0=ALU.mult), reads=["tmpC", "sel"], writes=["tmpC"])
            op("dve", I("scalar_tensor_tensor", out=bufA[:, q4 * 4:(q4 + 1) * 4, :], in0=v1, scalar=sel[:, 1:2], in1=v0, op0=ALU.mult, op1=ALU.add),
               reads=["tmpC", "xt", "sel"], writes=Akeys)
        for nb in range(8):
            wt, wk = W.load([(lambda t: t[:, :].rearrange("p (c n) -> p c n", c=16),
                              w_out[:, nb * 256:(nb + 1) * 256].rearrange("(c p) n -> p c n", p=128))])
            wv = wt[:, :].rearrange("p (c n) -> p c n", c=16)
            for tt in range(4):
                bank, hq = tt, 0
                for c in range(16):
                    op("pe", I("matmul", pb[bank][:, hq * 128:hq * 128 + 256], lhsT=bufA[:, c, tt * 128:(tt + 1) * 128], rhs=wv[:, c, :], start=(c == 0), stop=(c == 15)),
                       reads=[wk, Akeys[tt]], writes=[pq(bank, hq), pq(bank, hq + 1)])
                op("act", I("activation", out=bufB[:, tt, nb * 256:(nb + 1) * 256], in_=pb[bank][:, hq * 128:hq * 128 + 256], func=AF.Copy),
                   reads=[pq(bank, hq), pq(bank, hq + 1)], writes=[Bk(tt)])
        op("sp", I("dma_start", out=lnb[:, 0, :], in_=ln1[0:1, :].partition_broadcast(128)), writes=["lnb"], dma="lnb")
        op("sp", I("dma_start", out=lnb[:, 1, :], in_=ln1[1:2, :].partition_broadcast(128)), writes=["lnb"], dma="lnb")
        for tt in range(4):
            op("sp", I("dma_start", out=xt[:], in_=x(t0 + tt * 128)), reads=([xkey] if xkey else []), writes=["xt"], dma="xt")
            op("dve", I("scalar_tensor_tensor", out=bufB[:, tt, :], in0=xt[:], scalar=ALPHA, in1=bufB[:, tt, :], op0=ALU.mult, op1=ALU.add),
               reads=["xt", Bk(tt)], writes=[Bk(tt)])
            layer_norm_tile(P, bufB[:, tt, :], Bk(tt), tmpC, lnb, epsc, stat)
            for cg in range(4):
                bank = 6 + cg % 2
                for c4 in range(4):
                    c = cg * 4 + c4
                    op("pe", I("transpose", pb[bank][:, c4 * 128:(c4 + 1) * 128], tmpC[:, c * 128:(c + 1) * 128], ident[:]),
                       reads=["tmpC", "ident"], writes=[pq(bank, c4)])
                if cg % 2 == 0:
                    op("act", I("activation", out=bufA[:, cg * 4:(cg + 1) * 4, tt * 128:(tt + 1) * 128], in_=pb[bank][:, :].rearrange("p (c n) -> p c n", c=4), func=AF.Copy),
                       reads=pbank(bank), writes=[Akeys[tt]])
                else:
                    op("dve", I("tensor_copy", out=bufA[:, cg * 4:(cg + 1) * 4, tt * 128:(tt + 1) * 128], in_=pb[bank][:, :].rearrange("p (c n) -> p c n", c=4)),
                       reads=pbank(bank), writes=[Akeys[tt]])
            op("act", I("activation", out=bufB[:, tt, :], in_=tmpC[:], func=AF.Copy, scale=ALPHA), reads=["tmpC"], writes=[Bk(tt)])
            if DBG and st == 0 and tt == 0:
                op("sp", I("dma_start", out=dbg[0], in_=tmpC[:]), reads=["tmpC"], writes=["dbg"], dma="dbg")
            for c in range(16):
                op("pe", I("matmul", pb[5][:, 0:16], lhsT=bufA[:, c, tt * 128:(tt + 1) * 128], rhs=wr[:, c, :], start=(c == 0), stop=(c == 15)),
                   reads=[Akeys[tt], "wr"], writes=pbank(5))
            lg = rs[:, 0:16]
            op("dve", I("tensor_tensor", out=lg, in0=pb[5][:, 0:16], in1=brt[:], op=ALU.add), reads=pbank(5) + ["brt"], writes=["rs"])
            op("dve", I("tensor_reduce", out=rs[:, 16:17], in_=lg, axis=AX.X, op=ALU.max), reads=["rs"], writes=["rs"])
            op("dve", I("tensor_scalar", out=lg, in0=lg, scalar1=rs[:, 16:17], scalar2=None, op0=ALU.subtract), reads=["rs"], writes=["rs"])
            op("act", I("activation", out=lg, in_=lg, func=AF.Exp), reads=["rs"], writes=["rs"])
            pr3 = rs[:, 0:16].rearrange("p (g e) -> p g e", g=4)
            m1 = rs[:, 20:24]
            m2 = rs[:, 24:28]
            gsc = rs[:, 28:32]
            msk = rs[:, 32:48]
            msk3 = rs[:, 32:48].rearrange("p (g e) -> p g e", g=4)
            op("dve", I("tensor_reduce", out=m1, in_=pr3, axis=AX.X, op=ALU.max), reads=["rs"], writes=["rs"])
            op("dve", I("tensor_tensor", out=msk3, in0=pr3, in1=m1.unsqueeze(2).to_broadcast([128, 4, 4]), op=ALU.is_lt), reads=["rs"], writes=["rs"])
            op("dve", I("tensor_tensor", out=msk, in0=msk, in1=rs[:, 0:16], op=ALU.mult), reads=["rs"], writes=["rs"])
            op("dve", I("tensor_reduce", out=m2, in_=msk3, axis=AX.X, op=ALU.max), reads=["rs"], writes=["rs"])
            op("dve", I("tensor_tensor", out=gsc, in0=m1, in1=m2, op=ALU.add), reads=["rs"], writes=["rs"])
            op("dve", I("tensor_reduce", out=rs[:, 48:49], in_=gsc, axis=AX.X, op=ALU.max), reads=["rs"], writes=["rs"])
            op("dve", I("tensor_scalar", out=rs[:, 52:56], in0=gsc, scalar1=rs[:, 48:49], scalar2=None, op0=ALU.is_ge), reads=["rs"], writes=["rs"])
            op("dve", I("tensor_tensor", out=msk3, in0=pr3, in1=m2.unsqueeze(2).to_broadcast([128, 4, 4]), op=ALU.is_ge), reads=["rs"], writes=["rs"])
            op("dve", I("tensor_tensor", out=msk3, in0=msk3, in1=rs[:, 52:56].unsqueeze(2).to_broadcast([128, 4, 4]), op=ALU.mult), reads=["rs"], writes=["rs"])
            op("dve", I("tensor_tensor", out=msk, in0=msk, in1=rs[:, 0:16], op=ALU.mult), reads=["rs"], writes=["rs"])
            op("dve", I("reciprocal", out=rs[:, 49:50], in_=rs[:, 48:49]), reads=["rs"], writes=["rs"])
            op("dve", I("tensor_scalar", out=G[:, tt, :], in0=msk, scalar1=rs[:, 49:50], scalar2=None, op0=ALU.mult), reads=["rs"], writes=["G"])
        for e_ in range(NEXP):
            for hp in range(4):
                wgt, wgk = W.load([(lambda t: t[:, :].rearrange("p (c n) -> p c n", c=16),
                                    w_eg[e_, :, hp * 256:(hp + 1) * 256].rearrange("(c p) n -> p c n", p=128))])
                wut, wuk = W.load([(lambda t: t[:, :].rearrange("p (c n) -> p c n", c=16),
                                    w_eu[e_, :, hp * 256:(hp + 1) * 256].rearrange("(c p) n -> p c n", p=128))])
                wg = wgt[:, :].rearrange("p (c n) -> p c n", c=16)
                wu = wut[:, :].rearrange("p (c n) -> p c n", c=16)
                for half in range(2):
                    bg = half * 2
                    for c in range(16):
                        op("pe", I("matmul", pb[bg][:, :], lhsT=wg[:, c, half * 128:(half + 1) * 128], rhs=bufA[:, c, :], start=(c == 0), stop=(c == 15)), reads=[wgk] + Akeys, writes=pbank(bg))
                for half in range(2):
                    bu = half * 2 + 1
                    for c in range(16):
                        op("pe", I("matmul", pb[bu][:, :], lhsT=wu[:, c, half * 128:(half + 1) * 128], rhs=bufA[:, c, :], start=(c == 0), stop=(c == 15)), reads=[wuk] + Akeys, writes=pbank(bu))
                for half in range(2):
                    hbk = hp * 2 + half
                    bg, bu = half * 2, half * 2 + 1
                    sgt, sgk = sg[half], "sg%d" % half
                    op("act", I("activation", out=sgt[:], in_=pb[bg][:, :], func=AF.Silu), reads=pbank(bg), writes=[sgk])
                    op("dve", I("tensor_tensor", out=hid[:, hbk, :], in0=pb[bu][:, :], in1=sgt[:], op=ALU.mult), reads=pbank(bu) + [sgk], writes=["hid%d" % hbk])
            for nb in range(4):
                wt, wk = W.load([(lambda t: t[:, :].rearrange("p (c n) -> p c n", c=8),
                                  w_ed[e_, :, nb * 512:(nb + 1) * 512].rearrange("(c p) n -> p c n", p=128))])
                wd = wt[:, :].rearrange("p (c n) -> p c n", c=8)
                for tt in range(4):
                    bank = 4 + (nb * 4 + tt) % 2
                    for hbk in range(8):
                        op("pe", I("matmul", pb[bank][:, :], lhsT=hid[:, hbk, tt * 128:(tt + 1) * 128], rhs=wd[:, hbk, :], start=(hbk == 0), stop=(hbk == 7)),
                           reads=[wk, "hid%d" % hbk], writes=pbank(bank))
                    op("dve", I("scalar_tensor_tensor", out=bufB[:, tt, nb * 512:(nb + 1) * 512], in0=pb[bank][:, :], scalar=G[:, tt, e_:e_ + 1], in1=bufB[:, tt, nb * 512:(nb + 1) * 512], op0=ALU.mult, op1=ALU.add),
                       reads=pbank(bank) + ["G", Bk(tt)], writes=[Bk(tt)])
        if DBG and st == 0:
            op("sp", I("dma_start", out=dbg[1], in_=bufB[:, 0, :]), reads=[Bk(0)], writes=["dbg"], dma="dbg")
            op("sp", I("dma_start", out=dbg[2, :, 0:64], in_=G[:].rearrange("p a b -> p (a b)")), reads=["G"], writes=["dbg"], dma="dbg")
        op("sp", I("dma_start", out=lnb[:, 0, :], in_=ln2[0:1, :].partition_broadcast(128)), writes=["lnb"], dma="lnb")
        op("sp", I("dma_start", out=lnb[:, 1, :], in_=ln2[1:2, :].partition_broadcast(128)), writes=["lnb"], dma="lnb")
        op("sp", I("dma_start", out=ptok[:], in_=p[t0:t0 + 512, :].rearrange("(tt p) n -> p tt n", p=128)), writes=["ptok"], dma="ptok")
        for tt in range(4):
            layer_norm_tile(P, bufB[:, tt, :], Bk(tt), tmpC, lnb, epsc, stat)
            for cg in range(4):
                bank = 6 + cg % 2
                for c4 in range(4):
                    c = cg * 4 + c4
                    op("pe", I("transpose", pb[bank][:, c4 * 128:(c4 + 1) * 128], tmpC[:, c * 128:(c + 1) * 128], ident[:]),
                       reads=["tmpC", "ident"], writes=[pq(bank, c4)])
                if cg % 2 == 0:
                    op("act", I("activation", out=bufA[:, cg * 4:(cg + 1) * 4, tt * 128:(tt + 1) * 128], in_=pb[bank][:, :].rearrange("p (c n) -> p c n", c=4), func=AF.Copy),
                       reads=pbank(bank), writes=[Akeys[tt]])
                else:
                    op("dve", I("tensor_copy", out=bufA[:, cg * 4:(cg + 1) * 4, tt * 128:(tt + 1) * 128], in_=pb[bank][:, :].rearrange("p (c n) -> p c n", c=4)),
                       reads=pbank(bank), writes=[Akeys[tt]])
            op("act", I("activation", out=bufB[:, tt, :], in_=tmpC[:], func=AF.Copy), reads=["tmpC"], writes=[Bk(tt)])
            for c2 in range(2):
                op("pe", I("transpose", pb[5][:, c2 * 128:(c2 + 1) * 128], ptok[:, tt, c2 * 128:(c2 + 1) * 128], ident[:]), reads=["ptok", "ident"], writes=[pq(5, c2)])
            op("act", I("activation", out=pT[:, :, tt * 128:(tt + 1) * 128], in_=pb[5][:, 0:256].rearrange("p (c n) -> p c n", c=2), func=AF.Copy),
               reads=[pq(5, 0), pq(5, 1)], writes=["pT"])
        if DBG and st == 0:
            op("sp", I("dma_start", out=dbg[3], in_=bufB[:, 0, :]), reads=[Bk(0)], writes=["dbg"], dma="dbg")
        wpv, wpk = wppt[:], "wppt"
        for nb in range(8):
            wt, wk = W.load([(lambda t: t[:, :].rearrange("p (c n) -> p c n", c=16),
                              w_pg[:, nb * 256:(nb + 1) * 256].rearrange("(c p) n -> p c n", p=128))])
            wv = wt[:, :].rearrange("p (c n) -> p c n", c=16)
            for tt in range(4):
                bank = tt
                for c in range(16):
                    op("pe", I("matmul", pb[bank][:, 0:256], lhsT=bufA[:, c, tt * 128:(tt + 1) * 128], rhs=wv[:, c, :], start=(c == 0), stop=(c == 15)),
                       reads=[wk, Akeys[tt]], writes=[pq(bank, 0), pq(bank, 1)])
                for c2 in range(2):
                    op("pe", I("matmul", pb[bank][:, 256:512], lhsT=pT[:, c2, tt * 128:(tt + 1) * 128], rhs=wpv[:, c2, nb * 256:(nb + 1) * 256], start=(c2 == 0), stop=(c2 == 1)),
                       reads=[wpk, "pT"], writes=[pq(bank, 2), pq(bank, 3)])
                sgt, sgk = sg[tt % 2], "sg%d" % (tt % 2)
                op("act", I("activation", out=sgt[:, 0:256], in_=pb[bank][:, 0:256], func=AF.Sigmoid), reads=[pq(bank, 0), pq(bank, 1)], writes=[sgk])
                op("dve", I("tensor_tensor", out=sgt[:, 0:256], in0=pb[bank][:, 256:512], in1=sgt[:, 0:256], op=ALU.mult), reads=[pq(bank, 2), pq(bank, 3), sgk], writes=[sgk])
                op("dve", I("tensor_tensor", out=bufB[:, tt, nb * 256:(nb + 1) * 256], in0=bufB[:, tt, nb * 256:(nb + 1) * 256], in1=sgt[:, 0:256], op=ALU.add),
                   reads=[sgk, Bk(tt)], writes=[Bk(tt)])
        for tt in range(4):
            op("sp", I("dma_start", out=xo(t0 + tt * 128), in_=bufB[:, tt, :]), reads=[Bk(tt)], writes=["xo%d" % tt], dma="xo%d" % tt)
    P.wait_all("sp", ["xo%d" % tt for tt in range(4)])


def _consts():
    idx = np.arange(128)
    same = (idx[:, None] // 64) == (idx[None, :] // 64)
    c = {}
    c["ident"] = np.eye(128, dtype=np.float32)
    c["Umat"] = (same & (idx[:, None] <= idx[None, :])).astype(np.float32)
    c["NEGmat"] = np.where(same & (idx[None, :] >= idx[:, None]), 0.0, -30000.0).astype(np.float32)
    c["STRmat"] = (same & (idx[None, :] > idx[:, None])).astype(np.float32)
    c["BDmat"] = same.astype(np.float32)
    c["cmask"] = np.stack([(idx // 64 == 0), (idx // 64 == 1)], 1).astype(np.float32)
    return c


def _bands(win):
    tp = np.arange(128)[:, None]
    t = np.arange(128)[None, :]
    cur = np.where((tp <= t) & (tp > t - win), 1.0 / win, 0.0) - (tp == t)
    prev = np.where((tp - 128 > t - win), 1.0 / win, 0.0)
    cnt = np.minimum(t + 1, win).astype(np.float64)
    cur0 = np.where((tp <= t) & (tp > t - win), 1.0 / cnt, 0.0) - (tp == t)
    return cur.astype(np.float32), prev.astype(np.float32), cur0.astype(np.float32)


def stage_a_inputs(i, fh, xb, w_in, conv_w, a_log, dt_bias, dn_norm_w, pool_w, pool_scale):
    heads = [4 * fh + hl for hl in range(4)]
    cols = []
    for h in heads:
        for kind in range(4):
            cols.append(np.arange(kind * 1024 + h * 128, kind * 1024 + (h + 1) * 128))
    cols = np.concatenate(cols)
    m = {}
    if xb is not None:
        m["x"] = np.ascontiguousarray(xb)
    m["wA"] = np.ascontiguousarray(w_in[i][:, cols])
    ucols = np.concatenate([np.arange(4112 + (2 * fh + g) * 256, 4112 + (2 * fh + g + 1) * 256) for g in range(2)])
    m["wU"] = np.ascontiguousarray(w_in[i][:, ucols])
    bac = np.array([4096 + h for h in heads] + [4104 + h for h in heads])
    m["wba"] = np.ascontiguousarray(w_in[i][:, bac])
    cw = np.zeros((128, 12, 4), np.float32)
    for hl, h in enumerate(heads):
        for kind in range(3):
            cw[:, hl * 3 + kind, :] = conv_w[i][:, kind * 1024 + h * 128:kind * 1024 + (h + 1) * 128].T
    m["convw"] = cw
    m["negA"] = np.ascontiguousarray(np.broadcast_to(a_log[i][heads][None, :], (128, 4))).astype(np.float32)
    m["dtb"] = np.ascontiguousarray(np.broadcast_to(dt_bias[i][heads][None, :], (128, 4))).astype(np.float32)
    m["normw"] = np.ascontiguousarray(dn_norm_w[i][:, None]).astype(np.float32)
    m["poolw"] = np.ascontiguousarray(pool_w[i][2 * fh:2 * fh + 2])
    ps = np.zeros((128, 4), np.float32)
    for g in range(2):
        for oc in range(2):
            ps[:, g * 2 + oc] = pool_scale[i][(2 * fh + g) * 256 + oc * 128:(2 * fh + g) * 256 + (oc + 1) * 128]
    m["pscale"] = ps
    bc, bp, b0 = [], [], []
    for g in range(2):
        a, b, c = _bands(POOL_WINDOWS[2 * fh + g])
        bc.append(a); bp.append(b); b0.append(c)
    m["bcur"] = np.stack(bc); m["bprev"] = np.stack(bp); m["bcur0"] = np.stack(b0)
    m.update(_consts())
    return m


def stage_b_inputs(i, mixT_b, xb, pb_, w_out, ln1_g, ln1_b, w_router, b_router, w_e_gate, w_e_up, w_e_down,
                   ln2_g, ln2_b, w_ple_proj, w_ple_gate):
    m = {"p": np.ascontiguousarray(pb_)}
    m["w_out"] = w_out[i]
    m["ln1"] = np.stack([ln1_g[i], ln1_b[i]])
    m["ln2"] = np.stack([ln2_g[i], ln2_b[i]])
    m["w_router"] = w_router
    m["b_router"] = np.ascontiguousarray(b_router[None, :])
    m["w_eg"] = w_e_gate[i]
    m["w_eu"] = w_e_up[i]
    m["w_ed"] = w_e_down[i]
    m["w_pp"] = w_ple_proj[i]
    m["w_pg"] = w_ple_gate[i]
    m["ident"] = np.eye(128, dtype=np.float32)
    return m


def build_fused(S):
    nc = bass.Bass("TRN2", target_bir_lowering=False)
    H = S // 2
    NXC = H // 256
    groups = [[0, 1], [2, 3], [4, 5], [6, 7]]
    xfull = nc.dram_tensor("xfull", [S, D], F32, kind="ExternalInput").ap()
    xh = nc.dram_tensor("xh", [H, D], F32, kind="ExternalInput").ap()
    sel_d = nc.dram_tensor("sel", [128, 2], F32, kind="ExternalInput").ap()
    xo_ext = nc.dram_tensor("xo", [H, D], F32, kind="ExternalOutput").ap()
    MAt = [[nc.dram_tensor("MA%d_%d" % (h, k), [256, H], F32) for k in range(4)] for h in range(2)]
    Gt = [[nc.dram_tensor("G%d_%d" % (h, k), [512, H], F32) for k in range(4)] for h in range(2)]
    XOt = [nc.dram_tensor("XO_%d" % k, [256, D], F32) for k in range(NXC)]
    XGt = [nc.dram_tensor("XG_%d" % k, [512, D], F32) for k in range(NXC)]
    MA = [[t.ap() for t in MAt[h]] for h in range(2)]
    GM = [[t.ap() for t in Gt[h]] for h in range(2)]

    def x_ext_full(t):
        return xfull[t:t + 128, :]

    def x_ext_half(t):
        return xh[t:t + 128, :]

    def x_gathered(t):
        j, loc = t // H, t % H
        k, rr = loc // 256, loc % 256
        return XGt[k].ap()[j * 256 + rr:j * 256 + rr + 128, :]

    def x_own(t):
        return XOt[t // 256].ap()[t % 256:t % 256 + 128, :]

    def xo_final(t):
        return xo_ext[t:t + 128, :]

    P = Prog(nc)
    NDEP = int(os.environ.get("FUSE_DEPTH", str(DEPTH)))
    for i in range(NDEP):
        last = i == NDEP - 1
        stage_a(nc, P, S, "a%d_" % i, x_ext_full if i == 0 else x_gathered, None if i == 0 else "XG", MA)
        for h in range(2):
            for k in range(4):
                P.op("pool", I("collective_compute", "AllGather", ALU.bypass, replica_groups=groups, ins=[MAt[h][k].ap().opt()], outs=[Gt[h][k].ap().opt()]),
                     reads=["MA%dd" % h, "MA%dp" % h], writes=["G%d" % h], dma="cc", dinc=1)
        P.wait_all("pool", ["G0", "G1"])
        P.end_stage()
        stage_b(nc, P, H, "b%d_" % i, GM, sel_d, x_ext_half if i == 0 else x_own, None if i == 0 else "XO",
                xo_final if last else x_own, last)
        if not last:
            for k in range(NXC):
                P.op("pool", I("collective_compute", "AllGather", ALU.bypass, replica_groups=groups, ins=[XOt[k].ap().opt()], outs=[XGt[k].ap().opt()]),
                     reads=["xo%d" % tt for tt in range(4)], writes=["XG"], dma="cc", dinc=1)
            P.wait_all("pool", ["XG"])
        P.end_stage()
    P.finish()
    return nc


_NC_CACHE = {}
_SHK = {"bcur", "bprev", "bcur0", "ident", "Umat", "NEGmat", "STRmat", "BDmat", "cmask", "w_router", "b_router"}


def _perm_rows(w):
    idx = np.concatenate([np.arange(0, 512), np.arange(1024, 1536), np.arange(512, 1024), np.arange(1536, 2048)])
    return np.ascontiguousarray(w[idx])


def kernel(x, p, w_in, conv_w, a_log, dt_bias, dn_norm_w, pool_w, pool_scale, w_out,
           ln1_g, ln1_b, w_router, b_router, w_e_gate, w_e_up, w_e_down, ln2_g, ln2_b,
           w_ple_proj, w_ple_gate):
    f = lambda a: np.asarray(a, dtype=np.float32)
    (x, p, w_in, conv_w, a_log, dt_bias, dn_norm_w, pool_w, pool_scale, w_out, ln1_g, ln1_b, w_router, b_router,
     w_e_gate, w_e_up, w_e_down, ln2_g, ln2_b, w_ple_proj, w_ple_gate) = map(f, (
        x, p, w_in, conv_w, a_log, dt_bias, dn_norm_w, pool_w, pool_scale, w_out, ln1_g, ln1_b, w_router, b_router,
        w_e_gate, w_e_up, w_e_down, ln2_g, ln2_b, w_ple_proj, w_ple_gate))
    B, S, _ = x.shape
    H = S // 2
    if S not in _NC_CACHE:
        _NC_CACHE[S] = build_fused(S)
    nc = _NC_CACHE[S]
    w_out_p = [_perm_rows(w_out[i]) for i in range(DEPTH)]
    maps = []
    for c in range(8):
        b, r = c // 2, c % 2
        m = {"xfull": np.ascontiguousarray(x[b]), "xh": np.ascontiguousarray(x[b, r * H:(r + 1) * H])}
        sel = np.zeros((128, 2), np.float32)
        sel[:, r] = 1.0
        m["sel"] = sel
        for i in range(int(os.environ.get("FUSE_DEPTH", str(DEPTH)))):
            ma = stage_a_inputs(i, r, None, w_in, conv_w, a_log, dt_bias, dn_norm_w, pool_w, pool_scale)
            for k, v in ma.items():
                m[("c_" + k) if k in _SHK else "a%d_%s" % (i, k)] = v
            mb = stage_b_inputs(i, None, None, p[i, b, r * H:(r + 1) * H], w_out_p, ln1_g, ln1_b, w_router, b_router,
                                w_e_gate, w_e_up, w_e_down, ln2_g, ln2_b, w_ple_proj, w_ple_gate)
            for k, v in mb.items():
                m[("c_" + k) if k in _SHK else "b%d_%s" % (i, k)] = v
        maps.append(m)
    res = run_bass_kernel_spmd(nc, maps, core_ids=list(range(8)))
    out = np.stack([np.concatenate([res.results[2 * b]["xo"], res.results[2 * b + 1]["xo"]], 0) for b in range(B)])
    return out.astype(np.float32)
```
